# Optimizing a Trainium2 kernel written in Bass

```python
import math
import jax, jax.numpy as jnp
from jax import lax
import numpy as np

D_MODEL = 1024
BATCH = 4
SEQ = 4096
DEPTH = 4

CHUNK = 128
CONV_K = 4
NORM_EPS = 1e-6

ML_HEADS = 6
ML_HEAD_DIM = 128
ML_WIDTH = ML_HEADS * ML_HEAD_DIM
SSD_HEADS = 12
SSD_HEAD_DIM = 64
SSD_WIDTH = SSD_HEADS * SSD_HEAD_DIM
SSD_GROUPS = 2
SSD_STATE = 128
SSD_CONV_DIM = SSD_WIDTH + 2 * SSD_GROUPS * SSD_STATE
DT_MIN = 1e-3
DT_MAX = 1e-1
RET_HEADS = 4
RET_HEAD_DIM = 128
RET_WIDTH = RET_HEADS * RET_HEAD_DIM
ROPE_BASE = 10000.0
MAX_POS_OFFSET = 1024

MIX_WIDTH = ML_WIDTH + SSD_WIDTH + RET_WIDTH
ML_IN = 4 * ML_WIDTH + 2 * ML_HEADS
SSD_IN = SSD_WIDTH + SSD_CONV_DIM + SSD_HEADS
RET_IN = 4 * RET_WIDTH
IN_WIDTH = ML_IN + SSD_IN + RET_IN
FFN_HIDDEN = -(-8 * D_MODEL // (3 * 256)) * 256

kernel_name = "hymba_mlstm_ssd_retention_trunk"


def rmsnorm(x, g):
    xf = x.astype(jnp.float32)
    y = xf * lax.rsqrt(jnp.mean(xf * xf, axis=-1, keepdims=True) + NORM_EPS)
    return (y * g.astype(jnp.float32)).astype(x.dtype)


def group_norm(x, g, n_groups, center=False):
    shp = x.shape
    xf = x.astype(jnp.float32).reshape(*shp[:-1], n_groups, shp[-1] // n_groups)
    if center:
        xf = xf - jnp.mean(xf, axis=-1, keepdims=True)
    xf = xf * lax.rsqrt(jnp.mean(xf * xf, axis=-1, keepdims=True) + NORM_EPS)
    return (xf.reshape(shp) * g.astype(jnp.float32)).astype(x.dtype)


def causal_dwconv(x, w, b):
    y = lax.conv_general_dilated(
        x, w[:, None, :].astype(x.dtype), window_strides=(1,), padding=[(CONV_K - 1, 0)],
        dimension_numbers=('NWC', 'WIO', 'NWC'), feature_group_count=x.shape[-1])
    return y + b


def chunk_heads(t):
    t = t.reshape(t.shape[0], t.shape[1] // CHUNK, CHUNK, *t.shape[2:])
    return jnp.swapaxes(t, 2, 3)


def unchunk_heads(t):
    t = jnp.swapaxes(t, 2, 3)
    return t.reshape(t.shape[0], t.shape[1] * t.shape[2], *t.shape[3:])


def causal_mask():
    return jnp.tril(jnp.ones((CHUNK, CHUNK), dtype=bool))


def mlstm_chunkwise(q, k, v, i_pre, f_pre):
    f32 = jnp.float32
    d = q.shape[-1]
    qc = chunk_heads(q.astype(f32))
    kc = chunk_heads(k.astype(f32)) * (d ** -0.5)
    vc = chunk_heads(v.astype(f32))
    ig = chunk_heads(i_pre.astype(f32))
    lf = jax.nn.log_sigmoid(chunk_heads(f_pre.astype(f32)))
    b = jnp.cumsum(lf, axis=-1)
    b_last = b[..., -1]
    mask = causal_mask()
    log_d = jnp.where(mask, b[..., :, None] - b[..., None, :] + ig[..., None, :], -jnp.inf)
    w_state = b_last[..., None] - b + ig
    m_loc = jnp.max(w_state, axis=-1)
    e_state = jnp.exp(w_state - m_loc[..., None])
    c_loc = jnp.einsum('bchs,bchsk,bchsv->bchkv', e_state, kc, vc)
    n_loc = jnp.einsum('bchs,bchsk->bchk', e_state, kc)

    def step(carry, inp):
        c_st, n_st, m_st = carry
        c_l, n_l, m_l, bl = inp
        m_new = jnp.maximum(bl + m_st, m_l)
        a_old = jnp.exp(bl + m_st - m_new)
        a_loc = jnp.exp(m_l - m_new)
        c_new = a_old[..., None, None] * c_st + a_loc[..., None, None] * c_l
        n_new = a_old[..., None] * n_st + a_loc[..., None] * n_l
        return (c_new, n_new, m_new), (c_st, n_st, m_st)

    bsz, _, h = b_last.shape[0], b_last.shape[1], b_last.shape[2]
    init = (jnp.zeros((bsz, h, d, d), f32), jnp.zeros((bsz, h, d), f32), jnp.zeros((bsz, h), f32))
    xs = tuple(jnp.moveaxis(t, 1, 0) for t in (c_loc, n_loc, m_loc, b_last))
    _, (c_prev, n_prev, m_prev) = lax.scan(step, init, xs)
    c_prev = jnp.moveaxis(c_prev, 0, 1)
    n_prev = jnp.moveaxis(n_prev, 0, 1)
    m_prev = jnp.moveaxis(m_prev, 0, 1)

    a = b + m_prev[..., None]
    m_t = jnp.maximum(a, jnp.max(log_d, axis=-1))
    decay = jnp.exp(log_d - m_t[..., None])
    inter = jnp.exp(a - m_t)
    scores = jnp.einsum('bchtk,bchsk->bchts', qc, kc) * decay
    num = (jnp.einsum('bchts,bchsv->bchtv', scores, vc)
           + inter[..., None] * jnp.einsum('bchtk,bchkv->bchtv', qc, c_prev))
    den = jnp.sum(scores, axis=-1) + inter * jnp.einsum('bchtk,bchk->bcht', qc, n_prev)
    out = num / jnp.maximum(jnp.abs(den), jnp.exp(-m_t))[..., None]
    return unchunk_heads(out).astype(q.dtype)


def ssd_chunkwise(x, dt, a_neg, b_in, c_in):
    f32 = jnp.float32
    bsz, s, h, p = x.shape
    g = b_in.shape[2]
    hg = h // g
    nc = s // CHUNK
    xc = (x.astype(f32) * dt.astype(f32)[..., None]).reshape(bsz, nc, CHUNK, g, hg, p)
    a_c = (dt.astype(f32) * a_neg.astype(f32)).reshape(bsz, nc, CHUNK, g, hg)
    a_cum = jnp.cumsum(jnp.transpose(a_c, (0, 1, 3, 4, 2)), axis=-1)
    bc = b_in.astype(f32).reshape(bsz, nc, CHUNK, g, -1)
    cc = c_in.astype(f32).reshape(bsz, nc, CHUNK, g, -1)
    mask = causal_mask()
    l_dec = jnp.exp(jnp.where(mask, a_cum[..., :, None] - a_cum[..., None, :], -jnp.inf))
    cb = jnp.einsum('bclgn,bcsgn->bcgls', cc, bc)
    y_diag = jnp.einsum('bcgls,bcghls,bcsghp->bclghp', cb, l_dec, xc)
    decay_states = jnp.exp(a_cum[..., -1:] - a_cum)
    states = jnp.einsum('bcsgn,bcghs,bcsghp->bcghpn', bc, decay_states, xc)
    chunk_decay = jnp.exp(a_cum[..., -1])

    def step(carry, inp):
        st, dec = inp
        return dec[..., None, None] * carry + st, carry

    init = jnp.zeros((bsz, g, hg, p, bc.shape[-1]), f32)
    _, prev = lax.scan(step, init, (jnp.moveaxis(states, 1, 0), jnp.moveaxis(chunk_decay, 1, 0)))
    prev = jnp.moveaxis(prev, 0, 1)
    y_off = jnp.einsum('bclgn,bcghpn,bcghl->bclghp', cc, prev, jnp.exp(a_cum))
    return (y_diag + y_off).reshape(bsz, s, h, p).astype(x.dtype)


def retention_chunkwise(q, k, v, log_gamma):
    f32 = jnp.float32
    qc, kc, vc = (chunk_heads(t.astype(f32)) for t in (q, k, v))
    idx = jnp.arange(CHUNK, dtype=f32)
    mask = causal_mask()
    lg = log_gamma[:, None, None]
    d_mat = jnp.exp(jnp.where(mask, lg * (idx[:, None] - idx[None, :]), -jnp.inf))
    scores = jnp.einsum('bchtk,bchsk->bchts', qc, kc) * d_mat
    y_intra = jnp.einsum('bchts,bchsv->bchtv', scores, vc)
    zeta = jnp.exp(log_gamma[:, None] * (CHUNK - 1 - idx))
    xi = jnp.exp(log_gamma[:, None] * (idx + 1))
    states = jnp.einsum('bchsk,hs,bchsv->bchkv', kc, zeta, vc)
    chunk_decay = jnp.exp(log_gamma * CHUNK)[:, None, None]

    def step(carry, st):
        return chunk_decay * carry + st, carry

    init = jnp.zeros(states.shape[:1] + states.shape[2:], f32)
    _, r_prev = lax.scan(step, init, jnp.moveaxis(states, 1, 0))
    r_prev = jnp.moveaxis(r_prev, 0, 1)
    y_cross = jnp.einsum('bchtk,bchkv,ht->bchtv', qc, r_prev, xi)
    return unchunk_heads(y_intra + y_cross).astype(q.dtype)


def rope_tables(positions):
    inv_freq = ROPE_BASE ** (-jnp.arange(0, RET_HEAD_DIM, 2, dtype=jnp.float32) / RET_HEAD_DIM)
    ang = positions.astype(jnp.float32)[..., None] * inv_freq
    return jnp.cos(ang)[:, :, None, :], jnp.sin(ang)[:, :, None, :]


def apply_rope(t, cos, sin):
    tf = t.astype(jnp.float32)
    t1, t2 = jnp.split(tf, 2, axis=-1)
    return jnp.concatenate([t1 * cos - t2 * sin, t1 * sin + t2 * cos], axis=-1).astype(t.dtype)


def hybrid_layer(x, cos, sin, log_gamma, norm_mix, w_in, ml_conv_w, ml_conv_b, ml_gate_bias,
                 ml_norm, ssd_conv_w, ssd_conv_b, ssd_dt_bias, ssd_a_log, ssd_d, ssd_norm,
                 ret_norm, w_out, norm_ffn, w_gate_up, w_down):
    bsz, s, _ = x.shape
    h = rmsnorm(x, norm_mix)
    proj = h @ w_in
    ml_p, ssd_p, ret_p = jnp.split(proj, [ML_IN, ML_IN + SSD_IN], axis=-1)

    ml_qk, ml_v, ml_o, ml_if = jnp.split(ml_p, [2 * ML_WIDTH, 3 * ML_WIDTH, 4 * ML_WIDTH], axis=-1)
    ml_qk = jax.nn.silu(causal_dwconv(ml_qk, ml_conv_w, ml_conv_b))
    ml_q, ml_k = jnp.split(ml_qk, 2, axis=-1)
    i_pre, f_pre = jnp.split(ml_if + ml_gate_bias, 2, axis=-1)
    hml = mlstm_chunkwise(ml_q.reshape(bsz, s, ML_HEADS, ML_HEAD_DIM),
                          ml_k.reshape(bsz, s, ML_HEADS, ML_HEAD_DIM),
                          ml_v.reshape(bsz, s, ML_HEADS, ML_HEAD_DIM), i_pre, f_pre)
    y_ml = group_norm(hml.reshape(bsz, s, ML_WIDTH), ml_norm, ML_HEADS) * jax.nn.sigmoid(ml_o)

    z, xbc, dt_pre = jnp.split(ssd_p, [SSD_WIDTH, SSD_WIDTH + SSD_CONV_DIM], axis=-1)
    xbc = jax.nn.silu(causal_dwconv(xbc, ssd_conv_w, ssd_conv_b))
    xs, b_in, c_in = jnp.split(xbc, [SSD_WIDTH, SSD_WIDTH + SSD_GROUPS * SSD_STATE], axis=-1)
    dt = jax.nn.softplus(dt_pre + ssd_dt_bias)
    a_neg = -jnp.exp(ssd_a_log)
    xs_h = xs.reshape(bsz, s, SSD_HEADS, SSD_HEAD_DIM)
    y = ssd_chunkwise(xs_h, dt, a_neg, b_in.reshape(bsz, s, SSD_GROUPS, SSD_STATE),
                      c_in.reshape(bsz, s, SSD_GROUPS, SSD_STATE))
    y = y + ssd_d[:, None] * xs_h
    y_ssd = group_norm(y.reshape(bsz, s, SSD_WIDTH) * jax.nn.silu(z), ssd_norm, SSD_GROUPS)

    rq, rk, rv, rg = jnp.split(ret_p, 4, axis=-1)
    rq = apply_rope(rq.reshape(bsz, s, RET_HEADS, RET_HEAD_DIM), cos, sin)
    rk = apply_rope(rk.reshape(bsz, s, RET_HEADS, RET_HEAD_DIM), cos, sin) * (RET_HEAD_DIM ** -0.5)
    yr = retention_chunkwise(rq, rk, rv.reshape(bsz, s, RET_HEADS, RET_HEAD_DIM), log_gamma)
    y_ret = group_norm(yr.reshape(bsz, s, RET_WIDTH), ret_norm, RET_HEADS, center=True) * jax.nn.silu(rg)

    mix = jnp.concatenate([y_ml, y_ssd, y_ret], axis=-1)
    x = x + mix @ w_out

    h = rmsnorm(x, norm_ffn)
    gate, up = jnp.split(h @ w_gate_up, 2, axis=-1)
    return x + (jax.nn.silu(gate) * up) @ w_down


def setup_inputs(seed: int = 0) -> dict:
    key = jax.random.key(seed)
    ks = jax.random.split(key, 21)
    f32 = jnp.float32
    resid_scale = (2 * DEPTH) ** -0.5

    def nrm(k, shape, scale):
        return jax.random.normal(k, shape, f32) * scale

    x = nrm(ks[0], (BATCH, SEQ, D_MODEL), 1.0)
    offsets = jax.random.randint(ks[1], (BATCH, 1), 0, MAX_POS_OFFSET, dtype=jnp.int32)
    positions = offsets + jnp.arange(SEQ, dtype=jnp.int32)[None, :]
    norm_mix = 1.0 + nrm(ks[2], (DEPTH, D_MODEL), 0.02)
    w_in = nrm(ks[3], (DEPTH, D_MODEL, IN_WIDTH), D_MODEL ** -0.5)
    ml_conv_w = nrm(ks[4], (DEPTH, CONV_K, 2 * ML_WIDTH), CONV_K ** -0.5)
    ml_conv_b = nrm(ks[5], (DEPTH, 2 * ML_WIDTH), 0.02)
    i_bias = nrm(ks[6], (DEPTH, ML_HEADS), 0.5)
    f_bias = 3.0 + 3.0 * jax.random.uniform(ks[7], (DEPTH, ML_HEADS), f32)
    ml_gate_bias = jnp.concatenate([i_bias, f_bias], axis=-1)
    ml_norm = 1.0 + nrm(ks[8], (DEPTH, ML_WIDTH), 0.02)
    ssd_conv_w = nrm(ks[9], (DEPTH, CONV_K, SSD_CONV_DIM), CONV_K ** -0.5)
    ssd_conv_b = nrm(ks[10], (DEPTH, SSD_CONV_DIM), 0.02)
    dt0 = jnp.exp(jax.random.uniform(ks[11], (DEPTH, SSD_HEADS), f32)
                  * (math.log(DT_MAX) - math.log(DT_MIN)) + math.log(DT_MIN))
    ssd_dt_bias = dt0 + jnp.log(-jnp.expm1(-dt0))
    ssd_a_log = jnp.log(jax.random.uniform(ks[12], (DEPTH, SSD_HEADS), f32, minval=1.0, maxval=16.0))
    ssd_d = 1.0 + nrm(ks[13], (DEPTH, SSD_HEADS), 0.1)
    ssd_norm = 1.0 + nrm(ks[14], (DEPTH, SSD_WIDTH), 0.02)
    ret_norm = 1.0 + nrm(ks[15], (DEPTH, RET_WIDTH), 0.02)
    w_out = nrm(ks[16], (DEPTH, MIX_WIDTH, D_MODEL), MIX_WIDTH ** -0.5 * resid_scale)
    norm_ffn = 1.0 + nrm(ks[17], (DEPTH, D_MODEL), 0.02)
    w_gate_up = nrm(ks[18], (DEPTH, D_MODEL, 2 * FFN_HIDDEN), D_MODEL ** -0.5)
    w_down = nrm(ks[19], (DEPTH, FFN_HIDDEN, D_MODEL), FFN_HIDDEN ** -0.5 * resid_scale)
    norm_final = 1.0 + nrm(ks[20], (D_MODEL,), 0.02)
    return {"x": x, "positions": positions, "norm_mix": norm_mix, "w_in": w_in,
            "ml_conv_w": ml_conv_w, "ml_conv_b": ml_conv_b, "ml_gate_bias": ml_gate_bias,
            "ml_norm": ml_norm, "ssd_conv_w": ssd_conv_w, "ssd_conv_b": ssd_conv_b,
            "ssd_dt_bias": ssd_dt_bias, "ssd_a_log": ssd_a_log, "ssd_d": ssd_d,
            "ssd_norm": ssd_norm, "ret_norm": ret_norm, "w_out": w_out, "norm_ffn": norm_ffn,
            "w_gate_up": w_gate_up, "w_down": w_down, "norm_final": norm_final}


def reference(x, positions, norm_mix, w_in, ml_conv_w, ml_conv_b, ml_gate_bias, ml_norm,
              ssd_conv_w, ssd_conv_b, ssd_dt_bias, ssd_a_log, ssd_d, ssd_norm, ret_norm,
              w_out, norm_ffn, w_gate_up, w_down, norm_final):
    cos, sin = rope_tables(positions)
    log_gamma = jnp.log1p(-jnp.exp2(-5.0 - jnp.arange(RET_HEADS, dtype=jnp.float32)))
    for l in range(DEPTH):
        x = hybrid_layer(x, cos, sin, log_gamma, norm_mix[l], w_in[l], ml_conv_w[l], ml_conv_b[l],
                         ml_gate_bias[l], ml_norm[l], ssd_conv_w[l], ssd_conv_b[l],
                         ssd_dt_bias[l], ssd_a_log[l], ssd_d[l], ssd_norm[l], ret_norm[l],
                         w_out[l], norm_ffn[l], w_gate_up[l], w_down[l])
    return rmsnorm(x, norm_final)
```

```python
import math
from contextlib import ExitStack

import numpy as np
import concourse.bass as bass
import concourse.mybir as mybir
from concourse.bass_utils import run_bass_kernel_spmd

F32 = mybir.dt.float32
BF16 = mybir.dt.bfloat16
I32 = mybir.dt.int32
AF = mybir.ActivationFunctionType
ALU = mybir.AluOpType
AX = mybir.AxisListType

D = 1024
DEPTH = 4
NORM_EPS = 1e-6
ML_H, SSD_H, RET_H = 6, 12, 4
IN_WIDTH = 7192
FFN_H = 2816
NJ = FFN_H // 128
NEG = -30000.0
ENGS = ["pe", "dve", "act", "pool", "sp"]


import types


def _freeze(fn):
    if fn.__closure__ is None:
        return fn
    cells = []
    for c in fn.__closure__:
        try:
            cells.append(types.CellType(c.cell_contents))
        except ValueError:
            cells.append(c)
    return types.FunctionType(fn.__code__, fn.__globals__, fn.__name__, fn.__defaults__, tuple(cells))


class Buf:
    __slots__ = ("w", "rs", "excl")

    def __init__(self, excl=False):
        self.w = None
        self.rs = []
        self.excl = excl


class Chan:
    def __init__(self, sched, name):
        self.name = name
        self.sched = sched
        self.sems = [sched.new_sem(f"c_{name}_0")]
        self.count = 0


class Sched:
    EPOCH = 20000
    DEPOCH = 24000

    def __init__(self, nc):
        self.nc = nc
        self._cms = []
        self.ops = {e: [] for e in ENGS}
        self.cnt = {e: 0 for e in ENGS}
        self.sems = {e: [self.new_sem(f"s_{e}_0")] for e in ENGS}
        self.clock = {e: {} for e in ENGS}
        self.nops = {e: 0 for e in ENGS}

    def new_sem(self, name):
        cm = self.nc.semaphore(name)
        h = cm.__enter__()
        self._cms.append(cm)
        return h

    def close(self):
        for cm in reversed(self._cms):
            cm.__exit__(None, None, None)

    def _need(self, eng, tick):
        if tick is None:
            return
        key, ep, c, clk = tick
        if self.clock[eng].get(key, (-1, 0)) >= (ep, c):
            return
        sem = key.sems[ep] if isinstance(key, Chan) else self.sems[key][ep]
        self.ops[eng].append(lambda h, sem=sem, c=c: h.wait_ge(sem, c))
        my = self.clock[eng]
        my[key] = (ep, c)
        for k, v in clk.items():
            if my.get(k, (-1, 0)) < v:
                my[k] = v

    def _deps(self, eng, reads, writes):
        for r in reads:
            self._need(eng, r.w)
            if r.excl:
                for t in r.rs:
                    if t[0] != eng:
                        self._need(eng, t)
        for w in writes:
            if w.w is not None and w.w[0] != eng:
                self._need(eng, w.w)
            for t in w.rs:
                if t[0] != eng:
                    self._need(eng, t)

    def op(self, eng, fn, reads=(), writes=()):
        fn = _freeze(fn)
        self._deps(eng, reads, writes)
        if self.cnt[eng] >= self.EPOCH:
            self.sems[eng].append(self.new_sem(f"s_{eng}_{len(self.sems[eng])}"))
            self.cnt[eng] = 0
        self.cnt[eng] += 1
        self.nops[eng] += 1
        ep, c = len(self.sems[eng]) - 1, self.cnt[eng]
        sem = self.sems[eng][ep]
        self.ops[eng].append(lambda h, fn=fn, sem=sem: fn(h).then_inc(sem, 1))
        tick = (eng, ep, c, dict(self.clock[eng]))
        for w in writes:
            w.w = tick
            w.rs = []
        for r in reads:
            r.rs.append(tick)
        return tick

    def dma(self, q, chan, fn, reads=(), writes=()):
        fn = _freeze(fn)
        self._deps(q, reads, writes)
        if chan.count >= self.DEPOCH:
            chan.sems.append(self.new_sem(f"c_{chan.name}_{len(chan.sems)}"))
            chan.count = 0
        chan.count += 16
        ep, c = len(chan.sems) - 1, chan.count
        sem = chan.sems[ep]
        self.ops[q].append(lambda h, fn=fn, sem=sem: fn(h).then_inc(sem, 16))
        tick = (chan, ep, c, dict(self.clock[q]))
        for w in writes:
            w.w = tick
            w.rs = []
        for r in reads:
            r.rs.append(tick)
        return tick

    def wait_tick(self, eng, tick):
        self._need(eng, tick)

    def emit(self):
        nc = self.nc
        with nc.Block() as block:
            @block.tensor
            def _(h):
                for f in self.ops["pe"]:
                    f(h)

            @block.vector
            def _(h):
                for f in self.ops["dve"]:
                    f(h)

            @block.scalar
            def _(h):
                for f in self.ops["act"]:
                    f(h)

            @block.gpsimd
            def _(h):
                for f in self.ops["pool"]:
                    f(h)

            @block.sync
            def _(h):
                for f in self.ops["sp"]:
                    f(h)


O_MLQ, O_MLK, O_MLV, O_MLO, O_MLIF = 0, 768, 1536, 2304, 3072
O_SZ, O_SX, O_SB, O_SC, O_SDT = 3084, 3852, 4620, 4876, 5132
O_RQ, O_RK, O_RV, O_RG = 5144, 5656, 6168, 6680


def _r(a, n):
    return list(range(a, a + n))


IN_UNITS = [
    ("ml_fm0", "fm", _r(O_MLQ, 512)),
    ("ml_fm1", "fm", _r(O_MLQ + 512, 256) + _r(O_MLK, 256)),
    ("ml_fm2", "fm", _r(O_MLK + 256, 512)),
    ("ml_v0", "tm", _r(O_MLV, 384)),
    ("ml_v1", "tm", _r(O_MLV + 384, 384)),
    ("ml_o0", "tm", _r(O_MLO, 384)),
    ("ml_o1", "tm", _r(O_MLO + 384, 384) + _r(O_MLIF, 12)),
    ("sd_fm0", "fm", _r(O_SX, 512)),
    ("sd_fm1", "fm", _r(O_SX + 512, 256) + _r(O_SB, 256)),
    ("sd_fm2", "fm", _r(O_SC, 256)),
    ("sd_z0", "tm", _r(O_SZ, 384)),
    ("sd_z1", "tm", _r(O_SZ + 384, 384) + _r(O_SDT, 12)),
    ("rt_q", "tm", _r(O_RQ, 512)),
    ("rt_k", "tm", _r(O_RK, 512)),
    ("rt_v", "tm", _r(O_RV, 512)),
    ("rt_g", "tm", _r(O_RG, 512)),
]


def _unit_table():
    units = []
    off = 0
    for name, kind, cols in IN_UNITS:
        n = 8 * len(cols)
        units.append((name, n, off))
        off += 128 * n
    for i in range(4):
        n = 16 * 256
        units.append((f"out{i}", n, off))
        off += 128 * n
    for i in range(NJ // 2):
        n = 8 * 512
        units.append((f"gu{i}", n, off))
        off += 128 * n
    for i in range(8):
        n = NJ * 128
        units.append((f"dn{i}", n, off))
        off += 128 * n
    return units, off


UNITS, WL_TOTAL = _unit_table()
UNIT = {u[0]: u for u in UNITS}


def pack_weights(w_in, w_out, w_gu, w_down):
    parts = []
    for name, kind, cols in IN_UNITS:
        blk = w_in[:, cols]
        blk = blk.reshape(8, 128, len(cols)).transpose(1, 0, 2)
        parts.append(np.ascontiguousarray(blk).reshape(-1))
    for i in range(4):
        blk = w_out[:, i * 256:(i + 1) * 256].reshape(16, 128, 256).transpose(1, 0, 2)
        parts.append(np.ascontiguousarray(blk).reshape(-1))
    for i in range(NJ // 2):
        j0 = 2 * i
        cols = _r(j0 * 128, 256) + _r(FFN_H + j0 * 128, 256)
        blk = w_gu[:, cols].reshape(8, 128, 512).transpose(1, 0, 2)
        parts.append(np.ascontiguousarray(blk).reshape(-1))
    for i in range(8):
        blk = w_down[:, i * 128:(i + 1) * 128].reshape(NJ, 128, 128).transpose(1, 0, 2)
        parts.append(np.ascontiguousarray(blk).reshape(-1))
    out = np.concatenate(parts).astype(np.float32)
    assert out.size == WL_TOTAL
    return out


PA_GMIX, PA_GFFN, PA_CW, PA_CB, PA_MLB, PA_DTB, PA_ALOG, PA_DD = 0, 8, 16, 104, 126, 138, 150, 162
NPA = 174


def pack_params(l, norm_mix, ml_conv_w, ml_conv_b, ml_gate_bias, ml_norm, ssd_conv_w, ssd_conv_b,
                ssd_dt_bias, ssd_a_log, ssd_d, ssd_norm, ret_norm, norm_ffn):
    pa = np.zeros((128, NPA), np.float32)
    pa[:, PA_GMIX:PA_GMIX + 8] = norm_mix[l].reshape(8, 128).T
    pa[:, PA_GFFN:PA_GFFN + 8] = norm_ffn[l].reshape(8, 128).T
    cw = np.concatenate([ml_conv_w[l], ssd_conv_w[l]], axis=1)
    cb = np.concatenate([ml_conv_b[l], ssd_conv_b[l]], axis=0)
    pa[:, PA_CW:PA_CW + 88] = cw.reshape(4, 22, 128).transpose(2, 1, 0).reshape(128, 88)
    pa[:, PA_CB:PA_CB + 22] = cb.reshape(22, 128).T
    pa[:, PA_MLB:PA_MLB + 12] = ml_gate_bias[l][None, :]
    pa[:, PA_DTB:PA_DTB + 12] = ssd_dt_bias[l][None, :]
    pa[:, PA_ALOG:PA_ALOG + 12] = ssd_a_log[l][None, :]
    pa[:, PA_DD:PA_DD + 12] = ssd_d[l][None, :]
    gains = np.concatenate([ml_norm[l], ssd_norm[l], ret_norm[l]])[None, :]
    gains = np.ascontiguousarray(np.broadcast_to(gains, (128, 2048))).astype(np.float32)
    return pa, gains


C_ID, C_ONES, C_NONES, C_TRI, C_NTRI, C_NEGST, C_NEGTS, C_SELL = 0, 128, 256, 384, 512, 640, 1024, 1152
C_INVF, C_D2T, C_XI, C_ZETA, C_MISC = 1280, 1344, 1856, 1860, 1864
NCST = 1880
TWO_PI_HI = 6.28125
TWO_PI_LO = 2.0 * math.pi - 6.28125


def make_consts():
    c = np.zeros((128, NCST), np.float64)
    idx = np.arange(128)
    c[:, C_ID:C_ID + 128] = np.eye(128)
    c[:, C_ONES:C_ONES + 128] = 1.0
    c[:, C_NONES:C_NONES + 128] = -1.0
    tri = (idx[:, None] <= idx[None, :]).astype(np.float64)
    c[:, C_TRI:C_TRI + 128] = tri
    c[:, C_NTRI:C_NTRI + 128] = -tri
    negst = np.where(idx[:, None] <= idx[None, :], 0.0, NEG)
    c[:, C_NEGST:C_NEGST + 384] = np.tile(negst, (1, 3))
    c[:, C_NEGTS:C_NEGTS + 128] = np.where(idx[None, :] <= idx[:, None], 0.0, NEG)
    sel = np.zeros((128, 128))
    sel[127, :] = 1.0
    c[:, C_SELL:C_SELL + 128] = sel
    c[:, C_INVF:C_INVF + 64] = (10000.0 ** (-np.arange(0, 128, 2, dtype=np.float64) / 128.0))[None, :]
    lg = np.log1p(-np.exp2(-5.0 - np.arange(4, dtype=np.float64)))
    ksc = 128.0 ** -0.5
    for h in range(4):
        d2 = np.where(idx[:, None] <= idx[None, :], np.exp(-lg[h] * (idx[:, None] + 1.0)), 0.0) * ksc
        c[:, C_D2T + h * 128:C_D2T + (h + 1) * 128] = d2
        c[:, C_XI + h] = np.exp(lg[h] * (idx + 1.0))
        c[:, C_ZETA + h] = np.exp(lg[h] * (127.0 - idx)) * ksc
    c[:, C_MISC + 0] = 1.0
    c[:, C_MISC + 1] = NORM_EPS
    c[:, C_MISC + 2] = math.log(ksc)
    c[:, C_MISC + 3] = 0.0
    ret_cd = [float(np.exp(lg[h] * 128.0)) for h in range(4)]
    return c.astype(np.float32), ret_cd


CST_NP, RET_CD = make_consts()

ST_C, ST_M, ST_S, ST_R, ST_HALO = 0, 774, 780, 1548, 2060
NST = 2060 + 66


class Prog:
    def __init__(self, NT, n_phases=1, enable=("ml", "ssd", "ret", "ffn")):
        self.NT = NT
        self.T = NT * 512
        self.NPH = n_phases
        self.enable = enable
        self.nc = bass.Bass("TRN2", target_bir_lowering=False)
        self.es = ExitStack()
        self.S = Sched(self.nc)
        self.bufs = {}
        self.dumps = {}
        self.dbg_ticks = []
        self.debug = False

    def sb(self, name, shape, dt=F32):
        t = self.es.enter_context(self.nc.sbuf_tensor(name, shape, dt))
        return t

    def ps(self, name, shape, dt=F32):
        return self.es.enter_context(self.nc.psum_tensor(name, shape, dt))

    def dump(self, name, ap, bufs, dt=F32):
        if not getattr(self, "debug", False):
            return
        if name in self.dumps:
            return
        shape = list(ap.shape)
        d = self.nc.dram_tensor("dbg_" + name, shape, dt, kind="ExternalOutput").ap()
        self.dumps[name] = d
        chd = Chan(self.S, "dbg" + name)
        idx = tuple(slice(None) for _ in shape)
        self.dbg_ticks.append(self.S.dma("sp", chd, lambda h: h.dma_start(out=d[idx], in_=ap), reads=bufs))

    def B(self, name):
        b = self.bufs.get(name)
        if b is None:
            b = self.bufs[name] = Buf(excl=name[:2] in ("pa", "pm", "tb"))
        return b

    def build(self):
        nc, S, T, NT = self.nc, self.S, self.T, self.NT
        NCHK = T // 128
        dram = nc.dram_tensor
        xin = dram("xin", [8, 128, T], F32, kind="ExternalInput").ap()
        posd = dram("pos", [128, NCHK], I32, kind="ExternalInput").ap()
        wts = dram("wts", [self.NPH, WL_TOTAL], F32, kind="ExternalInput").ap()
        pad = dram("pa", [self.NPH, 128, NPA], F32, kind="ExternalInput").ap()
        gad = dram("gains", [self.NPH, 128, 2048], F32, kind="ExternalInput").ap()
        cstd = dram("cst", [128, NCST], F32, kind="ExternalInput").ap()
        nfd = dram("nfin", [128, 8], F32, kind="ExternalInput").ap()
        stin = dram("st_in", [128, NST], F32, kind="ExternalInput").ap()
        updd = dram("upd", [128, self.NPH], F32, kind="ExternalInput").ap()
        xout = dram("xout", [8, 128, T], F32, kind="ExternalOutput").ap()
        yout = dram("yout", [8, 128, T], F32, kind="ExternalOutput").ap()
        stout = dram("st_out", [128, NST], F32, kind="ExternalOutput").ap()
        wbf = dram("wbf", [self.NPH, WL_TOTAL], BF16).ap()
        self.wbf = wbf

        sb, ps, B = self.sb, self.ps, self.B
        xT = sb("xT", [128, 8, T])
        hT = sb("hT", [128, 8, 512], BF16)
        NW = 3
        wr = [sb(f"wr{i}", [128, 4096], BF16) for i in range(NW)]
        G = sb("G", [128, 28, 512], BF16)
        pc = [sb(f"pc{i}", [128, 515]) for i in range(2)]
        ctmp = sb("ctmp", [128, 512])
        mixT = sb("mixT", [128, 16, 512], BF16)
        prm = sb("prm", [128, NPA])
        gains = sb("gainsb", [128, 2048], BF16)
        cst = sb("cstt", [128, NCST])
        identb = sb("identb", [128, 128], BF16)
        nfin = sb("nfint", [128, 8])
        upd = sb("updt", [128, self.NPH])
        stt = sb("stt", [128, NST])
        Cbf = sb("Cbf", [128, 774], BF16)
        Sbf = sb("Sbf", [128, 768], BF16)
        Rbf = sb("Rbf", [128, 512], BF16)
        fA = sb("fA", [128, 774]); fB = sb("fB", [128, 774]); fC = sb("fC", [128, 774])
        hA = sb("hA", [128, 774], BF16); hB = sb("hB", [128, 774], BF16); hC = sb("hC", [128, 774], BF16)
        hD = sb("hD", [128, 256], BF16)
        hQ = sb("hQ", [128, 1024], BF16)
        mixtok = sb("mixtok", [128, 768], BF16)
        posi = sb("posi", [128, NCHK], I32)
        posf = sb("posf", [128, NCHK])
        cs2 = sb("cs2", [128, 128]); sn2 = sb("sn2", [128, 128])
        gml = [sb(f"gml{c}", [128, 12]) for c in range(4)]
        gdt = [sb(f"gdt{c}", [128, 12]) for c in range(4)]
        sm = sb("sm", [128, 256])
        aneg = sb("aneg", [128, 12])
        junk = sb("junk", [128, 512], BF16)
        pa_ = [ps(f"pa{i}", [128, 512]) for i in range(2)]
        pm = [ps(f"pm{i}", [128, 512]) for i in range(4)]
        tb = [ps(f"tb{i}", [128, 1024], BF16) for i in range(2)]

        cs = lambda o, n=128: cst[:, o:o + n]
        ident = cs(C_ID); ones = cs(C_ONES); nones = cs(C_NONES)
        one_c = cst[:, C_MISC:C_MISC + 1]; eps_c = cst[:, C_MISC + 1:C_MISC + 2]

        ch_ld = Chan(S, "ld")
        ch_cv = [Chan(S, f"cv{p}") for p in range(self.NPH)]
        ch_w = [Chan(S, f"w{i}") for i in range(NW)]
        ch_x = Chan(S, "x")
        ch_o = Chan(S, "o")
        ch_p = Chan(S, "p")
        ch_g = Chan(S, "g")

        S.dma("sp", ch_ld, lambda h: h.dma_start(out=cst[:], in_=cstd[:, :]), writes=[B("cst")])
        S.dma("sp", ch_ld, lambda h: h.dma_start(out=posi[:], in_=posd[:, :]), writes=[B("posi")])
        S.dma("sp", ch_ld, lambda h: h.dma_start(out=nfin[:], in_=nfd[:, :]), writes=[B("nfin")])
        S.dma("sp", ch_ld, lambda h: h.dma_start(out=upd[:], in_=updd[:, :]), writes=[B("upd")])
        S.dma("sp", ch_ld, lambda h: h.dma_start(out=stt[:], in_=stin[:, :]), writes=[B("stt"), B("halo")])
        for kc in range(8):
            S.dma("act", ch_x, lambda h, kc=kc: h.dma_start(out=xT[:, kc, :], in_=xin[kc, :, :]),
                  writes=[B(f"xT{kc}")])
        last = B("stt").w
        for n in ("cst", "posi", "nfin", "upd", "halo"):
            B(n).w = last
        lastx = B("xT7").w
        for kc in range(8):
            B(f"xT{kc}").w = lastx
        NCV = 8
        for p in range(self.NPH):
            step = WL_TOTAL // NCV
            assert step * NCV == WL_TOTAL
            for i in range(NCV):
                S.dma("pool", ch_cv[p],
                      lambda h, p=p, i=i: h.dma_start(
                          out=wbf[p, i * step:(i + 1) * step].rearrange("(a b) -> a b", a=128),
                          in_=wts[p, i * step:(i + 1) * step].rearrange("(a b) -> a b", a=128)),
                      writes=[B(f"wbf{p}")])
        S.op("dve", lambda h: h.tensor_copy(out=identb[:], in_=ident), reads=[B("cst")], writes=[B("identb")])
        S.op("dve", lambda h: h.tensor_copy(out=posf[:], in_=posi[:]), reads=[B("posi")], writes=[B("posf")])
        S.op("act", lambda h: h.copy(out=Cbf[:], in_=stt[:, ST_C:ST_C + 774]), reads=[B("stt")], writes=[B("Cbf")])
        S.op("act", lambda h: h.copy(out=Sbf[:], in_=stt[:, ST_S:ST_S + 768]), reads=[B("stt")], writes=[B("Sbf")])
        S.op("act", lambda h: h.copy(out=Rbf[:], in_=stt[:, ST_R:ST_R + 512]), reads=[B("stt")], writes=[B("Rbf")])

        self.wslot = 0

        def wload(p, uname):
            name, n, off = UNIT[uname]
            i = self.wslot % NW
            self.wslot += 1
            bw = B(f"wr{i}")
            S.dma("sp", ch_w[i],
                  lambda h, i=i, n=n, off=off, p=p: h.dma_start(
                      out=wr[i][:, 0:n],
                      in_=wbf[p, off:off + 128 * n].rearrange("(a b) -> a b", a=128)),
                  reads=[B(f"wbf{p}")], writes=[bw])
            return wr[i], bw

        def rmsnorm_tile(t0, gcol, eng_sq="act"):
            acc = pa_[0]
            for kc in range(8):
                S.op("act", lambda h, kc=kc: h.activation(out=junk[:], in_=xT[:, kc, t0:t0 + 512], func=AF.Square),
                     reads=[B(f"xT{kc}")], writes=[B("junk")])
                S.op("pe", lambda h, kc=kc: h.matmul(acc[:], lhsT=identb_ones[:], rhs=junk[:],
                                                      start=(kc == 0), stop=(kc == 7)),
                     reads=[B("junk"), B("onesb")], writes=[B("pa0")])
            S.op("act", lambda h: h.activation(out=ctmp[:], in_=acc[:], func=AF.Sqrt, bias=eps_c, scale=1.0 / D),
                 reads=[B("pa0"), B("cst")], writes=[B("ctmp")])
            S.op("dve", lambda h: h.reciprocal(out=ctmp[:], in_=ctmp[:]), reads=[B("ctmp")], writes=[B("ctmp")])
            for kc in range(8):
                S.op("dve", lambda h, kc=kc: h.scalar_tensor_tensor(
                    out=hT[:, kc, :], in0=xT[:, kc, t0:t0 + 512], scalar=prm[:, gcol + kc:gcol + kc + 1],
                    in1=ctmp[:], op0=ALU.mult, op1=ALU.mult),
                    reads=[B(f"xT{kc}"), B("prm"), B("ctmp")], writes=[B("hT")])

        identb_ones = sb("onesb", [128, 128], BF16)
        S.op("dve", lambda h: h.tensor_copy(out=identb_ones[:], in_=ones), reads=[B("cst")], writes=[B("onesb")])

        def bc(ap, shape):
            return ap.to_broadcast(shape)

        for p in range(self.NPH):
            S.dma("sp", ch_p, lambda h, p=p: h.dma_start(out=prm[:], in_=pad[p, :, :]), writes=[B("prm")])
            S.dma("pool", ch_g, lambda h, p=p: h.dma_start(out=gains[:], in_=gad[p, :, :]), writes=[B("gains")])
            S.op("act", lambda h: h.activation(out=aneg[:], in_=prm[:, PA_ALOG:PA_ALOG + 12], func=AF.Exp),
                 reads=[B("prm")], writes=[B("aneg")])
            S.op("dve", lambda h: h.tensor_scalar(out=aneg[:], in0=aneg[:], scalar1=-1.0, scalar2=0.0,
                                                  op0=ALU.mult, op1=ALU.add), reads=[B("aneg")], writes=[B("aneg")])
            updc = upd[:, p:p + 1]

            for ti in range(NT):
                t0 = ti * 512
                self.phase_tile(p, ti, t0, locals())

        self.finalize(locals())
        S.emit()
        S.close()
        self.es.close()
        return nc

    def phase_tile(self, p, ti, t0, L):
        S, B = self.S, self.B
        (xT, hT, G, pc, ctmp, mixT, prm, gains, cst, identb, stt, Cbf, Sbf, Rbf, fA, fB, fC, hA, hB, hC, hD, hQ,
         mixtok, posf, cs2, sn2, gml, gdt, sm, aneg, junk, pa_, pm, tb, wload, rmsnorm_tile, updc, bc) = [
            L[k] for k in ("xT", "hT", "G", "pc", "ctmp", "mixT", "prm", "gains", "cst", "identb", "stt", "Cbf",
                           "Sbf", "Rbf", "fA", "fB", "fC", "hA", "hB", "hC", "hD", "hQ", "mixtok", "posf", "cs2",
                           "sn2", "gml", "gdt", "sm", "aneg", "junk", "pa_", "pm", "tb", "wload", "rmsnorm_tile",
                           "updc", "bc")]
        cs = lambda o, n=128: cst[:, o:o + n]
        ident = cs(C_ID); ones = cs(C_ONES); nones = cs(C_NONES)
        one_c = cst[:, C_MISC:C_MISC + 1]; eps_c = cst[:, C_MISC + 1:C_MISC + 2]
        lnsc_c = cst[:, C_MISC + 2:C_MISC + 3]
        en = self.enable
        self.acc_i = 0

        def next_acc():
            i = self.acc_i % 2
            self.acc_i += 1
            return pa_[i], B(f"pa{i}")

        rmsnorm_tile(t0, PA_GMIX)

        def fm_unit(uname, blk0, slot0):
            name, n, off = UNIT[uname]
            W = n // 8
            wt, bw = wload(p, uname)
            wv = wt[:, 0:n].rearrange("p (k w) -> p k w", k=8)
            for j in range(W // 128):
                blk = blk0 + j
                slot = slot0 + j
                acc, bacc = next_acc()
                for kc in range(8):
                    S.op("pe", lambda h, kc=kc, j=j, acc=acc: h.matmul(
                        acc[:], lhsT=wv[:, kc, j * 128:(j + 1) * 128], rhs=hT[:, kc, :],
                        start=(kc == 0), stop=(kc == 7)), reads=[bw, B("hT")], writes=[bacc])
                pcb = pc[blk % 2]
                bpc = B(f"pc{blk % 2}")
                hal = stt[:, ST_HALO + 3 * blk:ST_HALO + 3 * blk + 3]
                S.op("pool", lambda h, pcb=pcb, hal=hal: h.tensor_copy(out=pcb[:, 0:3], in_=hal),
                     reads=[B("halo")], writes=[bpc])
                S.op("act", lambda h, pcb=pcb, acc=acc: h.copy(out=pcb[:, 3:515], in_=acc[:]),
                     reads=[bacc], writes=[bpc])
                S.op("pool", lambda h, pcb=pcb, hal=hal: h.tensor_copy(out=hal, in_=pcb[:, 512:515]),
                     reads=[bpc], writes=[B("halo")])
                cw = lambda tap, blk=blk: prm[:, PA_CW + blk * 4 + tap:PA_CW + blk * 4 + tap + 1]
                cb = prm[:, PA_CB + blk:PA_CB + blk + 1]
                S.op("act", lambda h, pcb=pcb, cw=cw, cb=cb: h.activation(
                    out=ctmp[:], in_=pcb[:, 0:512], func=AF.Identity, bias=cb, scale=cw(0)),
                    reads=[bpc, B("prm")], writes=[B("ctmp")])
                for tap in (1, 2, 3):
                    S.op("dve", lambda h, pcb=pcb, cw=cw, tap=tap: h.scalar_tensor_tensor(
                        out=ctmp[:], in0=pcb[:, tap:tap + 512], scalar=cw(tap), in1=ctmp[:],
                        op0=ALU.mult, op1=ALU.add), reads=[bpc, B("ctmp"), B("prm")], writes=[B("ctmp")])
                S.op("act", lambda h, slot=slot: h.activation(out=G[:, slot, :], in_=ctmp[:], func=AF.Silu),
                     reads=[B("ctmp")], writes=[B(f"G{slot}")])

        def tm_unit(uname, evac):
            name, n, off = UNIT[uname]
            W = n // 8
            wt, bw = wload(p, uname)
            wv = wt[:, 0:n].rearrange("p (k w) -> p k w", k=8)
            for c in range(4):
                acc, bacc = next_acc()
                for kc in range(8):
                    S.op("pe", lambda h, kc=kc, c=c, acc=acc: h.matmul(
                        acc[:, 0:W], lhsT=hT[:, kc, c * 128:(c + 1) * 128], rhs=wv[:, kc, :],
                        start=(kc == 0), stop=(kc == 7)), reads=[bw, B("hT")], writes=[bacc])
                evac(c, acc, bacc)

        def tmslot(c, q):
            return 12 + 4 * c + q

        def smc(i, n=6):
            return sm[:, i:i + n]

        if "ml" in en:
            fm_unit("ml_fm0", 0, 0)
            fm_unit("ml_fm1", 4, 4)
            fm_unit("ml_fm2", 8, 8)

            def vaug(c):
                return G[:, tmslot(c, 0):tmslot(c, 0) + 2, :].rearrange("p a b -> p (a b)")[:, 0:774].rearrange(
                    "p (h v) -> p h v", h=6)

            def ogt(c):
                return G[:, tmslot(c, 2):tmslot(c, 2) + 2, :].rearrange("p a b -> p (a b)")[:, 0:768]

            def ev_v(half):
                def f(c, acc, bacc):
                    bs = [B(f"G{tmslot(c, 0)}"), B(f"G{tmslot(c, 1)}")]
                    if half == 0:
                        S.op("pool", lambda h, c=c: h.memset(vaug(c)[:, :, 128:129], 1.0), writes=bs)
                    S.op("act", lambda h, c=c, acc=acc: h.copy(
                        out=vaug(c)[:, 3 * half:3 * half + 3, 0:128],
                        in_=acc[:, 0:384].rearrange("p (h v) -> p h v", h=3)), reads=[bacc] + bs, writes=bs)
                return f

            def ev_o(half):
                def f(c, acc, bacc):
                    bs = [B(f"G{tmslot(c, 2)}"), B(f"G{tmslot(c, 3)}")]
                    S.op("act", lambda h, acc=acc: h.activation(out=fA[:, 0:384], in_=acc[:, 0:384], func=AF.Sigmoid),
                         reads=[bacc], writes=[B("fA")])
                    if half == 1:
                        S.op("dve", lambda h, c=c, acc=acc: h.tensor_copy(out=gml[c][:], in_=acc[:, 384:396]),
                             reads=[bacc], writes=[B(f"gml{c}")])
                    S.op("pool", lambda h, c=c: h.tensor_tensor(
                        out=ogt(c)[:, 384 * half:384 * half + 384], in0=fA[:, 0:384],
                        in1=gains[:, 384 * half:384 * half + 384], op=ALU.mult),
                        reads=[B("fA"), B("gains")] + bs, writes=bs)
                return f

            tm_unit("ml_v0", ev_v(0))
            tm_unit("ml_v1", ev_v(1))
            tm_unit("ml_o0", ev_o(0))
            tm_unit("ml_o1", ev_o(1))
            for c in range(4):
                self.mlstm_chunk(c, t0, L, vaug(c), ogt(c), [B(f"G{tmslot(c, q)}") for q in range(4)])
        else:
            for mc in range(6):
                S.op("pool", lambda h, mc=mc: h.memset(mixT[:, mc, :], 0.0), writes=[B("mixT")])

        if "ssd" in en:
            fm_unit("sd_fm0", 12, 0)
            fm_unit("sd_fm1", 16, 4)
            fm_unit("sd_fm2", 20, 8)

            def zgt(c):
                return G[:, tmslot(c, 0):tmslot(c, 0) + 2, :].rearrange("p a b -> p (a b)")[:, 0:768]

            def ev_z(half):
                def f(c, acc, bacc):
                    bs = [B(f"G{tmslot(c, 0)}"), B(f"G{tmslot(c, 1)}")]
                    S.op("act", lambda h, c=c, acc=acc: h.activation(
                        out=zgt(c)[:, 384 * half:384 * half + 384], in_=acc[:, 0:384], func=AF.Silu),
                        reads=[bacc] + bs, writes=bs)
                    if half == 1:
                        S.op("dve", lambda h, c=c, acc=acc: h.tensor_copy(out=gdt[c][:], in_=acc[:, 384:396]),
                             reads=[bacc], writes=[B(f"gdt{c}")])
                return f

            tm_unit("sd_z0", ev_z(0))
            tm_unit("sd_z1", ev_z(1))
            for c in range(4):
                self.ssd_chunk(c, t0, L, zgt(c), [B(f"G{tmslot(c, q)}") for q in range(2)])
        else:
            for mc in range(6, 12):
                S.op("pool", lambda h, mc=mc: h.memset(mixT[:, mc, :], 0.0), writes=[B("mixT")])

        if "ret" in en:
            def rt(c, q):
                return G[:, tmslot(c, q), :]

            def rope_tables(c):
                gc = ti * 4 + c
                S.op("dve", lambda h, gc=gc: h.tensor_scalar(
                    out=fB[:, 0:64], in0=cs(C_INVF, 64), scalar1=posf[:, gc:gc + 1], scalar2=0.0,
                    op0=ALU.mult, op1=ALU.add), reads=[B("cst"), B("posf")], writes=[B("fB")])

                def reduce_sin(out_ap, shift, sign):
                    S.op("dve", lambda h: h.tensor_scalar(out=fB[:, 64:128], in0=fB[:, 0:64],
                                                          scalar1=1.0 / (2 * math.pi), scalar2=shift,
                                                          op0=ALU.mult, op1=ALU.add), reads=[B("fB")], writes=[B("fB")])
                    S.op("dve", lambda h: h.tensor_copy(out=fB[:, 128:192].bitcast(I32), in_=fB[:, 64:128]),
                         reads=[B("fB")], writes=[B("fB")])
                    S.op("dve", lambda h: h.tensor_copy(out=fB[:, 64:128], in_=fB[:, 128:192].bitcast(I32)),
                         reads=[B("fB")], writes=[B("fB")])
                    S.op("dve", lambda h: h.scalar_tensor_tensor(
                        out=fB[:, 128:192], in0=fB[:, 64:128], scalar=-TWO_PI_HI, in1=fB[:, 0:64],
                        op0=ALU.mult, op1=ALU.add), reads=[B("fB")], writes=[B("fB")])
                    S.op("dve", lambda h: h.scalar_tensor_tensor(
                        out=fB[:, 128:192], in0=fB[:, 64:128], scalar=-TWO_PI_LO, in1=fB[:, 128:192],
                        op0=ALU.mult, op1=ALU.add), reads=[B("fB")], writes=[B("fB")])
                    S.op("dve", lambda h: h.tensor_scalar(
                        out=fB[:, 128:192], in0=fB[:, 128:192], scalar1=shift * 2 * math.pi, scalar2=3.1415925,
                        op0=ALU.add, op1=ALU.min), reads=[B("fB")], writes=[B("fB")])
                    S.op("dve", lambda h: h.tensor_scalar(
                        out=fB[:, 128:192], in0=fB[:, 128:192], scalar1=-3.1415925, scalar2=0.0,
                        op0=ALU.max, op1=ALU.add), reads=[B("fB")], writes=[B("fB")])
                    S.op("act", lambda h: h.activation(out=out_ap, in_=fB[:, 128:192], func=AF.Sin, scale=sign),
                         reads=[B("fB")], writes=[B("rope")])

                reduce_sin(cs2[:, 0:64], 0.25, 1.0)
                S.op("pool", lambda h: h.tensor_copy(out=cs2[:, 64:128], in_=cs2[:, 0:64]),
                     reads=[B("rope")], writes=[B("rope")])
                reduce_sin(sn2[:, 64:128], 0.0, 1.0)
                S.op("pool", lambda h: h.tensor_scalar(out=sn2[:, 0:64], in0=sn2[:, 64:128], scalar1=-1.0,
                                                       scalar2=0.0, op0=ALU.mult, op1=ALU.add),
                     reads=[B("rope")], writes=[B("rope")])

            def ev_rope(q):
                def f(c, acc, bacc):
                    if q == 0:
                        rope_tables(c)
                    else:
                        pass
                    bs = [B(f"G{tmslot(c, q)}")]
                    a4 = acc[:].rearrange("p (h two j) -> p h two j", h=4, two=2)
                    S.op("dve", lambda h, acc=acc: h.tensor_tensor(
                        out=fA[:, 0:512].rearrange("p (h j) -> p h j", h=4),
                        in0=acc[:].rearrange("p (h j) -> p h j", h=4),
                        in1=bc(cs2[:].unsqueeze(1), [128, 4, 128]), op=ALU.mult),
                        reads=[bacc, B("rope")], writes=[B("fA")])
                    f4 = fC[:, 0:512].rearrange("p (h two j) -> p h two j", h=4, two=2)
                    S.op("dve", lambda h, a4=a4, f4=f4: h.tensor_tensor(
                        out=f4[:, :, 0, :], in0=a4[:, :, 1, :],
                        in1=bc(sn2[:, 0:64].unsqueeze(1), [128, 4, 64]), op=ALU.mult),
                        reads=[bacc, B("rope")], writes=[B("fC")])
                    S.op("dve", lambda h, a4=a4, f4=f4: h.tensor_tensor(
                        out=f4[:, :, 1, :], in0=a4[:, :, 0, :],
                        in1=bc(sn2[:, 64:128].unsqueeze(1), [128, 4, 64]), op=ALU.mult),
                        reads=[bacc, B("rope")], writes=[B("fC")])
                    S.op("pool", lambda h, c=c: h.tensor_tensor(out=rt(c, q), in0=fA[:, 0:512], in1=fC[:, 0:512],
                                                                op=ALU.add),
                         reads=[B("fA"), B("fC")], writes=bs)
                return f

            def ev_rope_k(c, acc, bacc):
                rope_tables(c)
                ev_rope(1)(c, acc, bacc)

            def ev_v(c, acc, bacc):
                S.op("act", lambda h, c=c, acc=acc: h.copy(out=rt(c, 2), in_=acc[:]), reads=[bacc],
                     writes=[B(f"G{tmslot(c, 2)}")])

            def ev_g(c, acc, bacc):
                S.op("act", lambda h, acc=acc: h.activation(out=fA[:, 0:512], in_=acc[:], func=AF.Silu),
                     reads=[bacc], writes=[B("fA")])
                S.op("pool", lambda h, c=c: h.tensor_tensor(out=rt(c, 3), in0=fA[:, 0:512], in1=gains[:, 1536:2048],
                                                            op=ALU.mult),
                     reads=[B("fA"), B("gains")], writes=[B(f"G{tmslot(c, 3)}")])

            tm_unit("rt_q", ev_rope(0))
            tm_unit("rt_k", ev_rope_k)
            tm_unit("rt_v", ev_v)
            tm_unit("rt_g", ev_g)
            for c in range(4):
                self.ret_chunk(c, t0, L, rt, [B(f"G{tmslot(c, q)}") for q in range(4)])
        else:
            for mc in range(12, 16):
                S.op("pool", lambda h, mc=mc: h.memset(mixT[:, mc, :], 0.0), writes=[B("mixT")])

        for i in range(4):
            wt, bw = wload(p, f"out{i}")
            wv = wt[:, 0:4096].rearrange("p (k w) -> p k w", k=16)
            for jj in range(2):
                db = 2 * i + jj
                acc, bacc = next_acc()
                for mc in range(16):
                    S.op("pe", lambda h, mc=mc, jj=jj, acc=acc: h.matmul(
                        acc[:], lhsT=wv[:, mc, jj * 128:(jj + 1) * 128], rhs=mixT[:, mc, :],
                        start=(mc == 0), stop=(mc == 15)), reads=[bw, B("mixT")], writes=[bacc])
                S.op("dve", lambda h, db=db, acc=acc: h.scalar_tensor_tensor(
                    out=xT[:, db, t0:t0 + 512], in0=acc[:], scalar=updc, in1=xT[:, db, t0:t0 + 512],
                    op0=ALU.mult, op1=ALU.add), reads=[bacc, B("upd"), B(f"xT{db}")], writes=[B(f"xT{db}")])

        if "ffn" in en:
            rmsnorm_tile(t0, PA_GFFN)
            for i in range(NJ // 2):
                wt, bw = wload(p, f"gu{i}")
                wv = wt[:, 0:4096].rearrange("p (k w) -> p k w", k=8)
                for jj in range(2):
                    j = 2 * i + jj
                    accg, bg = pa_[jj], B(f"pa{jj}")
                    accu, bu = pm[jj], B(f"pm{jj}")
                    for kc in range(8):
                        S.op("pe", lambda h, kc=kc, jj=jj, accg=accg: h.matmul(
                            accg[:], lhsT=wv[:, kc, jj * 128:(jj + 1) * 128], rhs=hT[:, kc, :],
                            start=(kc == 0), stop=(kc == 7)), reads=[bw, B("hT")], writes=[bg])
                    for kc in range(8):
                        S.op("pe", lambda h, kc=kc, jj=jj, accu=accu: h.matmul(
                            accu[:], lhsT=wv[:, kc, 256 + jj * 128:256 + (jj + 1) * 128], rhs=hT[:, kc, :],
                            start=(kc == 0), stop=(kc == 7)), reads=[bw, B("hT")], writes=[bu])
                    S.op("act", lambda h, accg=accg: h.activation(out=ctmp[:], in_=accg[:], func=AF.Silu),
                         reads=[bg], writes=[B("ctmp")])
                    S.op("dve", lambda h, accu=accu, j=j: h.tensor_tensor(out=G[:, j, :], in0=accu[:], in1=ctmp[:],
                                                                          op=ALU.mult),
                         reads=[bu, B("ctmp")], writes=[B(f"G{j}")])
            for db in range(8):
                wt, bw = wload(p, f"dn{db}")
                wv = wt[:, 0:NJ * 128].rearrange("p (k w) -> p k w", k=NJ)
                acc, bacc = next_acc()
                for j in range(NJ):
                    S.op("pe", lambda h, j=j, acc=acc: h.matmul(
                        acc[:], lhsT=wv[:, j, :], rhs=G[:, j, :], start=(j == 0), stop=(j == NJ - 1)),
                        reads=[bw, B(f"G{j}")], writes=[bacc])
                S.op("dve", lambda h, db=db, acc=acc: h.scalar_tensor_tensor(
                    out=xT[:, db, t0:t0 + 512], in0=acc[:], scalar=updc, in1=xT[:, db, t0:t0 + 512],
                    op0=ALU.mult, op1=ALU.add), reads=[bacc, B("upd"), B(f"xT{db}")], writes=[B(f"xT{db}")])

    def mix_transposes(self, L, mc0, nmc, src_ap, src_bufs, c, eng="act"):
        S, B = self.S, self.B
        tb, mixT, identb = L["tb"], L["mixT"], L["identb"]
        t = tb[1]
        for m in range(nmc):
            S.op("pe", lambda h, m=m: h.transpose(t[:, m * 128:(m + 1) * 128], src_ap[:, m * 128:(m + 1) * 128],
                                                  identb[:]),
                 reads=list(src_bufs) + [B("identb")], writes=[B("tb1")])
        S.op(eng, (lambda h: h.copy(out=mixT[:, mc0:mc0 + nmc, c * 128:(c + 1) * 128],
                                    in_=t[:, 0:nmc * 128].rearrange("p (m t) -> p m t", m=nmc))) if eng == "act" else
             (lambda h: h.tensor_copy(out=mixT[:, mc0:mc0 + nmc, c * 128:(c + 1) * 128],
                                      in_=t[:, 0:nmc * 128].rearrange("p (m t) -> p m t", m=nmc))),
             reads=[B("tb1")], writes=[B("mixT")])

    def mlstm_chunk(self, c, t0, L, vaug, og, gbufs):
        S, B = self.S, self.B
        (G, cst, identb, stt, Cbf, fA, fB, fC, hA, hB, hC, mixtok, gml, sm, pm, tb, prm, bc, junk) = [
            L[k] for k in ("G", "cst", "identb", "stt", "Cbf", "fA", "fB", "fC", "hA", "hB", "hC", "mixtok",
                           "gml", "sm", "pm", "tb", "prm", "bc", "junk")]
        cs = lambda o, n=128: cst[:, o:o + n]
        ident = cs(C_ID); ones = cs(C_ONES); nones = cs(C_NONES)
        one_c = cst[:, C_MISC:C_MISC + 1]; eps_c = cst[:, C_MISC + 1:C_MISC + 2]
        lnsc_c = cst[:, C_MISC + 2:C_MISC + 3]
        bsm = B("sm")
        bcst = B("cst")
        cc = slice(c * 128, (c + 1) * 128)
        qT = lambda h_: G[:, h_, cc]
        kT = lambda h_: G[:, 6 + h_, cc]
        bq = lambda h_: B(f"G{h_}")
        bk = lambda h_: B(f"G{6 + h_}")
        Cst = stt[:, ST_C:ST_C + 774]
        mprev = stt[:, ST_M:ST_M + 6]
        IFB, SP, BS, GG, CM, LAST, MM, INTER, MT_, EMT, GL, T1, T2, DEN, SC, ES, MLOC, BM, MNEW, AOLD, ALOC, KSF, SSQ = [
            sm[:, i * 6:(i + 1) * 6] for i in range(23)]
        IFB = sm[:, 0:12]; SP = sm[:, 12:18]; BS = sm[:, 18:24]; GG = sm[:, 24:30]; CM = sm[:, 30:36]
        LAST = sm[:, 36:48]; MM = sm[:, 48:54]; INTER = sm[:, 54:60]; MT_ = sm[:, 60:66]; EMT = sm[:, 66:72]
        GL = sm[:, 72:78]; T1 = sm[:, 78:84]; T2 = sm[:, 84:90]; DEN = sm[:, 90:96]; SC = sm[:, 96:102]
        ES = sm[:, 102:108]; MLOC = sm[:, 108:114]; BM = sm[:, 114:120]; MNEW = sm[:, 120:126]
        AOLD = sm[:, 126:132]; ALOC = sm[:, 132:138]; KSF = sm[:, 138:144]; SSQ = sm[:, 144:150]
        BCAT = sm[:, 18:36]

        def dv(fn, r=(), w=()):
            S.op("dve", fn, reads=[bsm] + list(r), writes=[bsm] + list(w))

        def ac(fn, r=(), w=()):
            S.op("act", fn, reads=[bsm] + list(r), writes=[bsm] + list(w))

        dv(lambda h: h.tensor_tensor(out=IFB, in0=gml[c][:], in1=prm[:, PA_MLB:PA_MLB + 12], op=ALU.add),
           r=[B(f"gml{c}"), B("prm")])
        ac(lambda h: h.activation(out=SP, in_=IFB[:, 6:12], func=AF.Exp, scale=-1.0))
        ac(lambda h: h.activation(out=SP, in_=SP, func=AF.Ln, bias=one_c, scale=1.0), r=[bcst])
        S.op("pe", lambda h: h.matmul(pm[3][:, 0:6], lhsT=cs(C_NTRI), rhs=SP, start=True, stop=True),
             reads=[bsm, bcst], writes=[B("pm3")])
        dv(lambda h: h.tensor_copy(out=BS, in_=pm[3][:, 0:6]), r=[B("pm3")])
        dv(lambda h: h.tensor_tensor(out=GG, in0=IFB[:, 0:6], in1=BS, op=ALU.subtract))
        S.op("dve", lambda h: h.tensor_tensor(
            out=fA[:, 0:768].rearrange("p (h s) -> p h s", h=6), in0=bc(ident.unsqueeze(1), [128, 6, 128]),
            in1=bc(GG.unsqueeze(2), [128, 6, 128]), op=ALU.mult), reads=[bsm, bcst], writes=[B("fA")])
        for hb in range(2):
            S.op("pe", lambda h, hb=hb: h.matmul(pm[hb][:, 0:384], lhsT=ones, rhs=fA[:, hb * 384:(hb + 1) * 384],
                                                 start=True, stop=True), reads=[B("fA"), bcst], writes=[B(f"pm{hb}")])
        for hb in range(2):
            S.op("dve", lambda h, hb=hb: h.tensor_tensor(
                out=fB[:, hb * 384:(hb + 1) * 384].rearrange("p (h s) -> p h s", h=3),
                in0=pm[hb][:, 0:384].rearrange("p (h s) -> p h s", h=3),
                in1=bc(cs(C_NEGTS).unsqueeze(1), [128, 3, 128]), op=ALU.add),
                reads=[B(f"pm{hb}"), bcst], writes=[B("fB")])
        dv(lambda h: h.tensor_reduce(out=CM, in_=fB[:, 0:768].rearrange("p (h s) -> p h s", h=6), axis=AX.X,
                                     op=ALU.max), r=[B("fB")])
        S.op("pe", lambda h: h.matmul(pm[3][:, 8:14], lhsT=cs(C_SELL), rhs=BS, start=True, stop=True),
             reads=[bsm, bcst], writes=[B("pm3")])
        S.op("pe", lambda h: h.matmul(pm[3][:, 16:22], lhsT=cs(C_SELL), rhs=CM, start=True, stop=True),
             reads=[bsm, bcst], writes=[B("pm3")])
        dv(lambda h: h.tensor_copy(out=LAST[:, 0:6], in_=pm[3][:, 8:14]), r=[B("pm3")])
        dv(lambda h: h.tensor_copy(out=LAST[:, 6:12], in_=pm[3][:, 16:22]), r=[B("pm3")])
        dv(lambda h: h.tensor_tensor(out=MM, in0=CM, in1=mprev, op=ALU.max), r=[B("stt")])
        dv(lambda h: h.tensor_tensor(out=INTER, in0=mprev, in1=MM, op=ALU.subtract), r=[B("stt")])
        ac(lambda h: h.activation(out=INTER, in_=INTER, func=AF.Exp))
        dv(lambda h: h.tensor_tensor(out=MT_, in0=BS, in1=MM, op=ALU.add))
        ac(lambda h: h.activation(out=EMT, in_=MT_, func=AF.Exp, scale=-1.0))
        dv(lambda h: h.tensor_scalar(out=GL, in0=GG, scalar1=lnsc_c, scalar2=0.0, op0=ALU.add, op1=ALU.add),
           r=[bcst])
        S.op("dve", lambda h: h.tensor_tensor(
            out=fA[:, 0:768].rearrange("p (h s) -> p h s", h=6), in0=bc(ident.unsqueeze(1), [128, 6, 128]),
            in1=bc(MM.unsqueeze(2), [128, 6, 128]), op=ALU.mult), reads=[bsm, bcst], writes=[B("fA")])
        for hb in range(2):
            S.op("pe", lambda h, hb=hb: h.matmul(pm[hb][:, 0:384], lhsT=nones, rhs=fA[:, hb * 384:(hb + 1) * 384],
                                                 start=True, stop=False), reads=[B("fA"), bcst], writes=[B(f"pm{hb}")])
            S.op("pe", lambda h, hb=hb: h.matmul(pm[hb][:, 0:384], lhsT=ident, rhs=cs(C_NEGST, 384),
                                                 start=False, stop=True), reads=[bcst], writes=[B(f"pm{hb}")])
        for h_ in range(6):
            S.op("act", lambda h, h_=h_: h.activation(
                out=hA[:, h_ * 128:(h_ + 1) * 128], in_=pm[h_ // 3][:, (h_ % 3) * 128:(h_ % 3 + 1) * 128],
                func=AF.Exp, bias=GL[:, h_:h_ + 1], scale=1.0), reads=[bsm, B(f"pm{h_ // 3}")], writes=[B("hA")])
        for h_ in range(6):
            S.op("pe", lambda h, h_=h_: h.matmul(pm[2 + h_ // 3][:, (h_ % 3) * 128:(h_ % 3 + 1) * 128],
                                                 lhsT=kT(h_), rhs=qT(h_), start=True, stop=True),
                 reads=[bq(h_), bk(h_)], writes=[B(f"pm{2 + h_ // 3}")])
        for hb in range(2):
            S.op("dve", lambda h, hb=hb: h.tensor_tensor(out=hB[:, hb * 384:(hb + 1) * 384], in0=pm[2 + hb][:, 0:384],
                                                         in1=hA[:, hb * 384:(hb + 1) * 384], op=ALU.mult),
                 reads=[B(f"pm{2 + hb}"), B("hA")], writes=[B("hB")])
        for h_ in range(6):
            S.op("pe", lambda h, h_=h_: h.matmul(pm[h_ // 3][:, (h_ % 3) * 129:(h_ % 3 + 1) * 129],
                                                 lhsT=hB[:, h_ * 128:(h_ + 1) * 128], rhs=vaug[:, h_, :],
                                                 start=True, stop=True),
                 reads=[B("hB"), gbufs[0], gbufs[1]], writes=[B(f"pm{h_ // 3}")])
        for h_ in range(6):
            S.op("pe", lambda h, h_=h_: h.matmul(pm[2 + h_ // 3][:, (h_ % 3) * 129:(h_ % 3 + 1) * 129],
                                                 lhsT=qT(h_), rhs=Cbf[:, h_ * 129:(h_ + 1) * 129],
                                                 start=True, stop=True),
                 reads=[bq(h_), B("Cbf")], writes=[B(f"pm{2 + h_ // 3}")])
        for hb in range(2):
            S.op("dve", lambda h, hb=hb: h.tensor_tensor(
                out=fB[:, hb * 387:(hb + 1) * 387].rearrange("p (h v) -> p h v", h=3),
                in0=pm[2 + hb][:, 0:387].rearrange("p (h v) -> p h v", h=3),
                in1=bc(INTER[:, hb * 3:hb * 3 + 3].unsqueeze(2), [128, 3, 129]), op=ALU.mult),
                reads=[B(f"pm{2 + hb}"), bsm], writes=[B("fB")])
            S.op("dve", lambda h, hb=hb: h.tensor_tensor(
                out=fC[:, hb * 387:(hb + 1) * 387], in0=pm[hb][:, 0:387], in1=fB[:, hb * 387:(hb + 1) * 387],
                op=ALU.add), reads=[B(f"pm{hb}"), B("fB")], writes=[B("fC")])
        nd = fC[:, 0:774].rearrange("p (h v) -> p h v", h=6)
        ac(lambda h: h.activation(out=DEN, in_=nd[:, :, 128], func=AF.Abs), r=[B("fC")])
        dv(lambda h: h.tensor_tensor(out=DEN, in0=DEN, in1=EMT, op=ALU.max))
        dv(lambda h: h.reciprocal(out=DEN, in_=DEN))
        dv(lambda h: h.memset(SSQ, 0.0))
        for h_ in range(6):
            S.op("act", lambda h, h_=h_: h.activation(out=junk[:, 0:128], in_=nd[:, h_, 0:128], func=AF.Square,
                                                      accum_out=SSQ[:, h_:h_ + 1]),
                 reads=[B("fC"), bsm], writes=[B("junk"), bsm])
        dv(lambda h: h.tensor_tensor(out=T1, in0=DEN, in1=DEN, op=ALU.mult))
        dv(lambda h: h.tensor_tensor(out=T1, in0=T1, in1=SSQ, op=ALU.mult))
        ac(lambda h: h.activation(out=T1, in_=T1, func=AF.Sqrt, bias=eps_c, scale=1.0 / 128.0), r=[bcst])
        dv(lambda h: h.reciprocal(out=T1, in_=T1))
        dv(lambda h: h.tensor_tensor(out=SC, in0=T1, in1=DEN, op=ALU.mult))
        for h_ in range(6):
            S.op("dve", lambda h, h_=h_: h.scalar_tensor_tensor(
                out=mixtok[:, h_ * 128:(h_ + 1) * 128], in0=nd[:, h_, 0:128], scalar=SC[:, h_:h_ + 1],
                in1=og[:, h_ * 128:(h_ + 1) * 128], op0=ALU.mult, op1=ALU.mult),
                reads=[B("fC"), bsm, gbufs[2], gbufs[3]], writes=[B("mixtok")])
        self.mix_transposes(L, 0, 6, mixtok, [B("mixtok")], c)
        dv(lambda h: h.tensor_tensor(out=ES, in0=GG, in1=LAST[:, 6:12], op=ALU.subtract))
        ac(lambda h: h.activation(out=ES, in_=ES, func=AF.Exp))
        dv(lambda h: h.tensor_tensor(out=MLOC, in0=LAST[:, 0:6], in1=LAST[:, 6:12], op=ALU.add))
        dv(lambda h: h.tensor_tensor(out=BM, in0=LAST[:, 0:6], in1=mprev, op=ALU.add), r=[B("stt")])
        dv(lambda h: h.tensor_tensor(out=MNEW, in0=BM, in1=MLOC, op=ALU.max))
        dv(lambda h: h.tensor_tensor(out=AOLD, in0=BM, in1=MNEW, op=ALU.subtract))
        ac(lambda h: h.activation(out=AOLD, in_=AOLD, func=AF.Exp))
        dv(lambda h: h.tensor_tensor(out=ALOC, in0=MLOC, in1=MNEW, op=ALU.subtract))
        ac(lambda h: h.activation(out=ALOC, in_=ALOC, func=AF.Exp, bias=lnsc_c, scale=1.0), r=[bcst])
        dv(lambda h: h.tensor_tensor(out=KSF, in0=ES, in1=ALOC, op=ALU.mult))
        for h_ in range(6):
            S.op("pe", lambda h, h_=h_: h.transpose(tb[0][:, h_ * 128:(h_ + 1) * 128], kT(h_), identb[:]),
                 reads=[bk(h_), B("identb")], writes=[B("tb0")])
        S.op("dve", lambda h: h.tensor_tensor(
            out=hC[:, 0:768].rearrange("p (h k) -> p h k", h=6),
            in0=tb[0][:, 0:768].rearrange("p (h k) -> p h k", h=6),
            in1=bc(KSF.unsqueeze(2), [128, 6, 128]), op=ALU.mult), reads=[B("tb0"), bsm], writes=[B("hC")])
        for h_ in range(6):
            S.op("pe", lambda h, h_=h_: h.matmul(pm[h_ // 3][:, (h_ % 3) * 129:(h_ % 3 + 1) * 129],
                                                 lhsT=hC[:, h_ * 128:(h_ + 1) * 128], rhs=vaug[:, h_, :],
                                                 start=True, stop=True),
                 reads=[B("hC"), gbufs[0], gbufs[1]], writes=[B(f"pm{h_ // 3}")])
        S.op("dve", lambda h: h.tensor_tensor(
            out=Cst.rearrange("p (h v) -> p h v", h=6), in0=Cst.rearrange("p (h v) -> p h v", h=6),
            in1=bc(AOLD.unsqueeze(2), [128, 6, 129]), op=ALU.mult), reads=[bsm, B("stt"), B("Cbf")], writes=[B("stt")])
        for hb in range(2):
            S.op("dve", lambda h, hb=hb: h.tensor_tensor(
                out=Cst[:, hb * 387:(hb + 1) * 387], in0=Cst[:, hb * 387:(hb + 1) * 387], in1=pm[hb][:, 0:387],
                op=ALU.add), reads=[B(f"pm{hb}"), B("stt")], writes=[B("stt")])
        S.op("act", lambda h: h.copy(out=Cbf[:], in_=Cst), reads=[B("stt")], writes=[B("Cbf")])
        dv(lambda h: h.tensor_copy(out=mprev, in_=MNEW), r=[B("stt")], w=[B("stt")])

    def ssd_chunk(self, c, t0, L, zg, gbufs):
        S, B = self.S, self.B
        (G, cst, identb, stt, Sbf, fA, fB, fC, hA, hB, hC, hD, mixtok, gdt, sm, pm, tb, prm, bc, junk, aneg,
         gains) = [L[k] for k in ("G", "cst", "identb", "stt", "Sbf", "fA", "fB", "fC", "hA", "hB", "hC", "hD",
                                  "mixtok", "gdt", "sm", "pm", "tb", "prm", "bc", "junk", "aneg", "gains")]
        cs = lambda o, n=128: cst[:, o:o + n]
        ident = cs(C_ID); ones = cs(C_ONES)
        one_c = cst[:, C_MISC:C_MISC + 1]; eps_c = cst[:, C_MISC + 1:C_MISC + 2]
        bsm, bcst = B("sm"), B("cst")
        cc = slice(c * 128, (c + 1) * 128)
        xTb = lambda j: G[:, j, cc]
        BT = lambda g: G[:, 6 + g, cc]
        CT = lambda g: G[:, 8 + g, cc]
        Sst = stt[:, ST_S:ST_S + 768]
        DT = sm[:, 0:12]; ACS = sm[:, 12:24]; NACS = sm[:, 24:36]; ACL = sm[:, 36:48]; EA = sm[:, 48:60]
        DEC = sm[:, 60:72]; CD = sm[:, 72:84]; SSQ = sm[:, 84:86]; RS = sm[:, 86:88]; TMP = sm[:, 88:100]

        def dv(fn, r=(), w=()):
            S.op("dve", fn, reads=[bsm] + list(r), writes=[bsm] + list(w))

        def ac(fn, r=(), w=()):
            S.op("act", fn, reads=[bsm] + list(r), writes=[bsm] + list(w))

        dv(lambda h: h.tensor_tensor(out=DT, in0=gdt[c][:], in1=prm[:, PA_DTB:PA_DTB + 12], op=ALU.add),
           r=[B(f"gdt{c}"), B("prm")])
        ac(lambda h: h.activation(out=DT, in_=DT, func=AF.Exp))
        ac(lambda h: h.activation(out=DT, in_=DT, func=AF.Ln, bias=one_c, scale=1.0), r=[bcst])
        dv(lambda h: h.tensor_tensor(out=TMP, in0=DT, in1=aneg[:], op=ALU.mult), r=[B("aneg")])
        S.op("pe", lambda h: h.matmul(pm[3][:, 0:12], lhsT=cs(C_TRI), rhs=TMP, start=True, stop=True),
             reads=[bsm, bcst], writes=[B("pm3")])
        dv(lambda h: h.tensor_copy(out=ACS, in_=pm[3][:, 0:12]), r=[B("pm3")])
        dv(lambda h: h.tensor_scalar(out=NACS, in0=ACS, scalar1=-1.0, scalar2=0.0, op0=ALU.mult, op1=ALU.add))
        S.op("pe", lambda h: h.matmul(pm[3][:, 16:28], lhsT=cs(C_SELL), rhs=ACS, start=True, stop=True),
             reads=[bsm, bcst], writes=[B("pm3")])
        dv(lambda h: h.tensor_copy(out=ACL, in_=pm[3][:, 16:28]), r=[B("pm3")])
        ac(lambda h: h.activation(out=EA, in_=ACS, func=AF.Exp))
        dv(lambda h: h.tensor_tensor(out=DEC, in0=ACL, in1=ACS, op=ALU.subtract))
        ac(lambda h: h.activation(out=DEC, in_=DEC, func=AF.Exp))
        ac(lambda h: h.activation(out=CD, in_=ACL, func=AF.Exp))
        for j in range(6):
            S.op("pe", lambda h, j=j: h.transpose(tb[0][:, j * 128:(j + 1) * 128], xTb(j), identb[:]),
                 reads=[B(f"G{j}"), B("identb")], writes=[B("tb0")])
        for g in range(2):
            S.op("pe", lambda h, g=g: h.transpose(tb[0][:, 768 + g * 128:768 + (g + 1) * 128], BT(g), identb[:]),
                 reads=[B(f"G{6 + g}"), B("identb")], writes=[B("tb0")])
        xtok = tb[0][:, 0:768].rearrange("p (h q) -> p h q", h=12)
        S.op("dve", lambda h: h.tensor_tensor(out=hC[:, 0:768].rearrange("p (h q) -> p h q", h=12), in0=xtok,
                                              in1=bc(DT.unsqueeze(2), [128, 12, 64]), op=ALU.mult),
             reads=[B("tb0"), bsm], writes=[B("hC")])
        S.op("dve", lambda h: h.tensor_tensor(out=fB[:, 0:768].rearrange("p (h q) -> p h q", h=12), in0=xtok,
                                              in1=bc(prm[:, PA_DD:PA_DD + 12].unsqueeze(2), [128, 12, 64]),
                                              op=ALU.mult),
             reads=[B("tb0"), B("prm")], writes=[B("fB")])
        S.op("act", lambda h: h.copy(out=hD[:], in_=tb[0][:, 768:1024]), reads=[B("tb0")], writes=[B("hD")])
        for g in range(2):
            S.op("pe", lambda h, g=g: h.matmul(pm[3][:, 128 + g * 128:256 + g * 128], lhsT=BT(g), rhs=CT(g),
                                               start=True, stop=True),
                 reads=[B(f"G{6 + g}"), B(f"G{8 + g}")], writes=[B("pm3")])
        for g in range(2):
            S.op("dve", lambda h, g=g: h.tensor_tensor(
                out=fA[:, 0:768].rearrange("p (h s) -> p h s", h=6), in0=bc(ident.unsqueeze(1), [128, 6, 128]),
                in1=bc(ACS[:, 6 * g:6 * g + 6].unsqueeze(2), [128, 6, 128]), op=ALU.mult),
                reads=[bsm, bcst], writes=[B("fA")])
            for hb in range(2):
                S.op("pe", lambda h, hb=hb: h.matmul(pm[hb][:, 0:384], lhsT=ones, rhs=fA[:, hb * 384:(hb + 1) * 384],
                                                     start=True, stop=False),
                     reads=[B("fA"), bcst], writes=[B(f"pm{hb}")])
                S.op("pe", lambda h, hb=hb: h.matmul(pm[hb][:, 0:384], lhsT=ident, rhs=cs(C_NEGST, 384),
                                                     start=False, stop=True), reads=[bcst], writes=[B(f"pm{hb}")])
            for hh in range(6):
                S.op("act", lambda h, hh=hh, g=g: h.activation(
                    out=hA[:, hh * 128:(hh + 1) * 128], in_=pm[hh // 3][:, (hh % 3) * 128:(hh % 3 + 1) * 128],
                    func=AF.Exp, bias=NACS[:, 6 * g + hh:6 * g + hh + 1], scale=1.0),
                    reads=[bsm, B(f"pm{hh // 3}")], writes=[B("hA")])
            S.op("dve", lambda h, g=g: h.tensor_tensor(
                out=hB[:, 0:768].rearrange("p (h l) -> p h l", h=6),
                in0=bc(pm[3][:, 128 + g * 128:256 + g * 128].unsqueeze(1), [128, 6, 128]),
                in1=hA[:, 0:768].rearrange("p (h l) -> p h l", h=6), op=ALU.mult),
                reads=[B("pm3"), B("hA")], writes=[B("hB")])
            ydst = pm[2] if g == 0 else L["pa_"][1]
            ybuf = B("pm2") if g == 0 else B("pa1")
            for hh in range(6):
                hd = 6 * g + hh
                S.op("pe", lambda h, hh=hh, hd=hd, ydst=ydst: h.matmul(
                    ydst[:, hh * 64:(hh + 1) * 64], lhsT=hB[:, hh * 128:(hh + 1) * 128],
                    rhs=hC[:, hd * 64:(hd + 1) * 64], start=True, stop=True),
                    reads=[B("hB"), B("hC")], writes=[ybuf])
            S.op("dve", lambda h, g=g, ydst=ydst: h.tensor_tensor(
                out=fC[:, g * 384:(g + 1) * 384], in0=ydst[:, 0:384], in1=fB[:, g * 384:(g + 1) * 384], op=ALU.add),
                reads=[ybuf, B("fB")], writes=[B("fC")])
        for g in range(2):
            S.op("pe", lambda h, g=g: h.matmul(pm[g][:, 0:384], lhsT=CT(g), rhs=Sbf[:, g * 384:(g + 1) * 384],
                                               start=True, stop=True),
                 reads=[B(f"G{8 + g}"), B("Sbf")], writes=[B(f"pm{g}")])
            S.op("dve", lambda h, g=g: h.tensor_tensor(
                out=fA[:, g * 384:(g + 1) * 384].rearrange("p (h q) -> p h q", h=6),
                in0=pm[g][:, 0:384].rearrange("p (h q) -> p h q", h=6),
                in1=bc(EA[:, 6 * g:6 * g + 6].unsqueeze(2), [128, 6, 64]), op=ALU.mult),
                reads=[B(f"pm{g}"), bsm], writes=[B("fA")])
        S.op("pool", lambda h: h.tensor_tensor(out=fC[:, 0:768], in0=fC[:, 0:768], in1=fA[:, 0:768], op=ALU.add),
             reads=[B("fA"), B("fC")], writes=[B("fC")])
        S.op("pool", lambda h: h.tensor_tensor(out=fC[:, 0:768], in0=fC[:, 0:768], in1=zg, op=ALU.mult),
             reads=[B("fC"), gbufs[0], gbufs[1]], writes=[B("fC")])
        dv(lambda h: h.memset(SSQ, 0.0))
        for g in range(2):
            S.op("act", lambda h, g=g: h.activation(out=junk[:, 0:384], in_=fC[:, g * 384:(g + 1) * 384],
                                                    func=AF.Square, accum_out=SSQ[:, g:g + 1]),
                 reads=[B("fC"), bsm], writes=[B("junk"), bsm])
        ac(lambda h: h.activation(out=RS, in_=SSQ, func=AF.Sqrt, bias=eps_c, scale=1.0 / 384.0), r=[bcst])
        dv(lambda h: h.reciprocal(out=RS, in_=RS))
        for g in range(2):
            S.op("dve", lambda h, g=g: h.scalar_tensor_tensor(
                out=mixtok[:, g * 384:(g + 1) * 384], in0=fC[:, g * 384:(g + 1) * 384], scalar=RS[:, g:g + 1],
                in1=gains[:, 768 + g * 384:768 + (g + 1) * 384], op0=ALU.mult, op1=ALU.mult),
                reads=[B("fC"), bsm, B("gains")], writes=[B("mixtok")])
        self.mix_transposes(L, 6, 6, mixtok, [B("mixtok")], c)
        S.op("pool", lambda h: h.tensor_tensor(
            out=hA[:, 0:768].rearrange("p (h q) -> p h q", h=12), in0=hC[:, 0:768].rearrange("p (h q) -> p h q", h=12),
            in1=bc(DEC.unsqueeze(2), [128, 12, 64]), op=ALU.mult), reads=[B("hC"), bsm], writes=[B("hA")])
        for g in range(2):
            S.op("pe", lambda h, g=g: h.matmul(pm[g][:, 0:384], lhsT=hD[:, g * 128:(g + 1) * 128],
                                               rhs=hA[:, g * 384:(g + 1) * 384], start=True, stop=True),
                 reads=[B("hD"), B("hA")], writes=[B(f"pm{g}")])
        S.op("dve", lambda h: h.tensor_tensor(
            out=Sst.rearrange("p (h q) -> p h q", h=12), in0=Sst.rearrange("p (h q) -> p h q", h=12),
            in1=bc(CD.unsqueeze(2), [128, 12, 64]), op=ALU.mult), reads=[bsm, B("stt"), B("Sbf")], writes=[B("stt")])
        for g in range(2):
            S.op("dve", lambda h, g=g: h.tensor_tensor(
                out=Sst[:, g * 384:(g + 1) * 384], in0=Sst[:, g * 384:(g + 1) * 384], in1=pm[g][:, 0:384],
                op=ALU.add), reads=[B(f"pm{g}"), B("stt")], writes=[B("stt")])
        S.op("act", lambda h: h.copy(out=Sbf[:], in_=Sst), reads=[B("stt")], writes=[B("Sbf")])

    def ret_chunk(self, c, t0, L, rt, gbufs):
        S, B = self.S, self.B
        (G, cst, identb, stt, Rbf, fA, fB, fC, hA, hB, hC, hQ, mixtok, sm, pm, tb, bc, junk) = [
            L[k] for k in ("G", "cst", "identb", "stt", "Rbf", "fA", "fB", "fC", "hA", "hB", "hC", "hQ", "mixtok",
                           "sm", "pm", "tb", "bc", "junk")]
        cs = lambda o, n=128: cst[:, o:o + n]
        eps_c = cst[:, C_MISC + 1:C_MISC + 2]
        bsm, bcst = B("sm"), B("cst")
        Rst = stt[:, ST_R:ST_R + 512]
        SUM = sm[:, 0:4]; SSQ = sm[:, 4:8]; MEAN = sm[:, 8:12]; VAR = sm[:, 12:16]; SCL = sm[:, 16:20]
        T1 = sm[:, 20:24]
        XI = cs(C_XI, 4)

        def dv(fn, r=(), w=()):
            S.op("dve", fn, reads=[bsm] + list(r), writes=[bsm] + list(w))

        def ac(fn, r=(), w=()):
            S.op("act", fn, reads=[bsm] + list(r), writes=[bsm] + list(w))

        rq, rk, rv, gg = rt(c, 0), rt(c, 1), rt(c, 2), rt(c, 3)
        if c == 0 and t0 == 0:
            self.dump("rq", rq, [gbufs[0]], BF16); self.dump("rk", rk, [gbufs[1]], BF16)
            self.dump("rv", rv, [gbufs[2]], BF16); self.dump("gg", gg, [gbufs[3]], BF16)
            self.dump("cs2", L["cs2"][:], [B("rope")]); self.dump("sn2", L["sn2"][:], [B("rope")])
            self.dump("hT", L["hT"][:, 0, :], [B("hT")], BF16)
        for h_ in range(4):
            S.op("pe", lambda h, h_=h_: h.transpose(tb[0][:, h_ * 128:(h_ + 1) * 128], rq[:, h_ * 128:(h_ + 1) * 128],
                                                    identb[:]), reads=[gbufs[0], B("identb")], writes=[B("tb0")])
        for h_ in range(4):
            S.op("pe", lambda h, h_=h_: h.transpose(tb[0][:, 512 + h_ * 128:512 + (h_ + 1) * 128],
                                                    rk[:, h_ * 128:(h_ + 1) * 128], identb[:]),
                 reads=[gbufs[1], B("identb")], writes=[B("tb0")])
        S.op("act", lambda h: h.copy(out=hQ[:], in_=tb[0][:]), reads=[B("tb0")], writes=[B("hQ")])
        qT = lambda h_: hQ[:, h_ * 128:(h_ + 1) * 128]
        kT = lambda h_: hQ[:, 512 + h_ * 128:512 + (h_ + 1) * 128]
        for h_ in range(4):
            S.op("pe", lambda h, h_=h_: h.matmul(pm[2][:, h_ * 128:(h_ + 1) * 128], lhsT=kT(h_), rhs=qT(h_),
                                                 start=True, stop=True), reads=[B("hQ")], writes=[B("pm2")])
        S.op("dve", lambda h: h.tensor_tensor(out=hC[:, 0:512], in0=pm[2][:], in1=cs(C_D2T, 512), op=ALU.mult),
             reads=[B("pm2"), bcst], writes=[B("hC")])
        for h_ in range(4):
            S.op("pe", lambda h, h_=h_: h.matmul(pm[0][:, h_ * 128:(h_ + 1) * 128],
                                                 lhsT=hC[:, h_ * 128:(h_ + 1) * 128], rhs=rv[:, h_ * 128:(h_ + 1) * 128],
                                                 start=True, stop=False), reads=[B("hC"), gbufs[2]], writes=[B("pm0")])
            S.op("pe", lambda h, h_=h_: h.matmul(pm[0][:, h_ * 128:(h_ + 1) * 128], lhsT=qT(h_),
                                                 rhs=Rbf[:, h_ * 128:(h_ + 1) * 128], start=False, stop=True),
                 reads=[B("hQ"), B("Rbf")], writes=[B("pm0")])
        dv(lambda h: h.memset(sm[:, 0:8], 0.0))
        for h_ in range(4):
            S.op("act", lambda h, h_=h_: h.activation(out=fC[:, h_ * 128:(h_ + 1) * 128],
                                                      in_=pm[0][:, h_ * 128:(h_ + 1) * 128], func=AF.Identity,
                                                      accum_out=SUM[:, h_:h_ + 1]),
                 reads=[B("pm0"), bsm], writes=[B("fC"), bsm])
            S.op("act", lambda h, h_=h_: h.activation(out=junk[:, 0:128], in_=pm[0][:, h_ * 128:(h_ + 1) * 128],
                                                      func=AF.Square, accum_out=SSQ[:, h_:h_ + 1]),
                 reads=[B("pm0"), bsm], writes=[B("junk"), bsm])
        dv(lambda h: h.tensor_scalar(out=MEAN, in0=SUM, scalar1=1.0 / 128.0, scalar2=0.0, op0=ALU.mult, op1=ALU.add))
        dv(lambda h: h.tensor_tensor(out=T1, in0=MEAN, in1=MEAN, op=ALU.mult))
        dv(lambda h: h.scalar_tensor_tensor(out=VAR, in0=SSQ, scalar=1.0 / 128.0, in1=T1, op0=ALU.mult,
                                            op1=ALU.subtract))
        dv(lambda h: h.tensor_tensor(out=VAR, in0=VAR, in1=XI, op=ALU.mult), r=[bcst])
        dv(lambda h: h.tensor_tensor(out=VAR, in0=VAR, in1=XI, op=ALU.mult), r=[bcst])
        ac(lambda h: h.activation(out=VAR, in_=VAR, func=AF.Sqrt, bias=eps_c, scale=1.0), r=[bcst])
        dv(lambda h: h.reciprocal(out=VAR, in_=VAR))
        dv(lambda h: h.tensor_tensor(out=SCL, in0=VAR, in1=XI, op=ALU.mult), r=[bcst])
        for h_ in range(4):
            S.op("dve", lambda h, h_=h_: h.tensor_scalar(
                out=fA[:, h_ * 128:(h_ + 1) * 128], in0=fC[:, h_ * 128:(h_ + 1) * 128], scalar1=MEAN[:, h_:h_ + 1],
                scalar2=SCL[:, h_:h_ + 1], op0=ALU.subtract, op1=ALU.mult),
                reads=[B("fC"), bsm], writes=[B("fA")])
        S.op("pool", lambda h: h.tensor_tensor(out=mixtok[:, 0:512], in0=fA[:, 0:512], in1=gg, op=ALU.mult),
             reads=[B("fA"), gbufs[3]], writes=[B("mixtok")])
        if c == 0 and t0 == 0:
            self.dump("rety", fC[:, 0:512], [B("fC")]); self.dump("retn", fA[:, 0:512], [B("fA")])
            self.dump("retsm", sm[:, 0:24], [bsm]); self.dump("retmix", mixtok[:, 0:512], [B("mixtok")], BF16)
        self.mix_transposes(L, 12, 4, mixtok, [B("mixtok")], c)
        S.op("pool", lambda h: h.tensor_tensor(
            out=hA[:, 0:512].rearrange("p (h k) -> p h k", h=4), in0=rk.rearrange("p (h k) -> p h k", h=4),
            in1=bc(cs(C_ZETA, 4).unsqueeze(2), [128, 4, 128]), op=ALU.mult),
            reads=[gbufs[1], bcst], writes=[B("hA")])
        for h_ in range(4):
            S.op("pe", lambda h, h_=h_: h.matmul(pm[1][:, h_ * 128:(h_ + 1) * 128],
                                                 lhsT=hA[:, h_ * 128:(h_ + 1) * 128], rhs=rv[:, h_ * 128:(h_ + 1) * 128],
                                                 start=True, stop=True), reads=[B("hA"), gbufs[2]], writes=[B("pm1")])
        for h_ in range(4):
            S.op("dve", lambda h, h_=h_: h.scalar_tensor_tensor(
                out=Rst[:, h_ * 128:(h_ + 1) * 128], in0=Rst[:, h_ * 128:(h_ + 1) * 128], scalar=RET_CD[h_],
                in1=pm[1][:, h_ * 128:(h_ + 1) * 128], op0=ALU.mult, op1=ALU.add),
                reads=[B("pm1"), B("stt"), B("Rbf")], writes=[B("stt")])
        S.op("act", lambda h: h.copy(out=Rbf[:], in_=Rst), reads=[B("stt")], writes=[B("Rbf")])

    def finalize(self, L):
        S, B = self.S, self.B
        xT, stt, nfin, ctmp, junk, pa_, cst = [L[k] for k in ("xT", "stt", "nfin", "ctmp", "junk", "pa_", "cst")]
        xout, yout, stout, ch_o = L["xout"], L["yout"], L["stout"], L["ch_o"]
        onesb = L["identb_ones"]
        fA = L["fA"]
        eps_c = cst[:, C_MISC + 1:C_MISC + 2]
        T = self.T
        ticks = []
        ticks2 = [None, None]
        ch_o2 = [Chan(S, "o2a"), Chan(S, "o2b")]
        ticks.append(S.dma("sp", ch_o, lambda h: h.dma_start(out=stout[:, :], in_=stt[:]), reads=[B("stt"), B("halo")]))
        for kc in range(8):
            ticks.append(S.dma("sp", ch_o, lambda h, kc=kc: h.dma_start(out=xout[kc, :, :], in_=xT[:, kc, :]),
                               reads=[B(f"xT{kc}")]))
        ytile = L["hQ"]
        stage = [L["pc"][0], L["pc"][1]]
        for ti in range(self.NT):
            t0 = ti * 512
            acc = pa_[0]
            for kc in range(8):
                S.op("act", lambda h, kc=kc: h.activation(out=junk[:], in_=xT[:, kc, t0:t0 + 512], func=AF.Square),
                     reads=[B(f"xT{kc}")], writes=[B("junk")])
                S.op("pe", lambda h, kc=kc: h.matmul(acc[:], lhsT=onesb[:], rhs=junk[:], start=(kc == 0),
                                                     stop=(kc == 7)), reads=[B("junk"), B("onesb")], writes=[B("pa0")])
            S.op("act", lambda h: h.activation(out=ctmp[:], in_=acc[:], func=AF.Sqrt, bias=eps_c, scale=1.0 / D),
                 reads=[B("pa0"), B("cst")], writes=[B("ctmp")])
            S.op("dve", lambda h: h.reciprocal(out=ctmp[:], in_=ctmp[:]), reads=[B("ctmp")], writes=[B("ctmp")])
            for kc in range(8):
                st_ = stage[kc % 2]
                bst = B(f"pc{kc % 2}")
                S.op("dve", lambda h, kc=kc, st_=st_: h.scalar_tensor_tensor(
                    out=st_[:, 0:512], in0=xT[:, kc, t0:t0 + 512], scalar=nfin[:, kc:kc + 1], in1=ctmp[:],
                    op0=ALU.mult, op1=ALU.mult), reads=[B(f"xT{kc}"), B("nfin"), B("ctmp")], writes=[bst])
                ticks2[kc % 2] = S.dma("sp", ch_o2[kc % 2], lambda h, kc=kc, st_=st_: h.dma_start(
                    out=yout[kc, :, t0:t0 + 512], in_=st_[:, 0:512]), reads=[bst])
        S.wait_tick("sp", ticks[-1])
        S.wait_tick("sp", ticks2[0])
        S.wait_tick("sp", ticks2[1])
        for t in self.dbg_ticks:
            S.wait_tick("sp", t)


_PROG_CACHE = {}


DEBUG = False


def get_prog(NT, n_phases, enable=("ml", "ssd", "ret", "ffn")):
    key = (NT, n_phases, tuple(enable))
    if key not in _PROG_CACHE:
        pr = Prog(NT, n_phases, enable)
        pr.debug = DEBUG
        _PROG_CACHE[key] = pr.build()
    return _PROG_CACHE[key]


def run_layers_unfused(x, positions, layer_w, layer_pa, layer_ga, norm_final, enable=("ml", "ssd", "ret", "ffn")):
    Bt, Sq, _ = x.shape
    T = Sq // 2
    NT = T // 512
    ncores = 2 * Bt
    depth = len(layer_w)
    nc = get_prog(NT, 1, enable)
    nfin = np.ascontiguousarray(norm_final.reshape(8, 128).T).astype(np.float32)
    xs = []
    poss = []
    for b in range(Bt):
        for half in range(2):
            seg = x[b, half * T:(half + 1) * T, :]
            xs.append(np.ascontiguousarray(seg.T.reshape(8, 128, T)).astype(np.float32))
            pp = positions[b, half * T:(half + 1) * T].reshape(T // 128, 128).T
            poss.append(np.ascontiguousarray(pp).astype(np.int32))
    st_zero = np.zeros((128, NST), np.float32)
    st_prev = [st_zero for _ in range(Bt)]
    ys = [None] * ncores
    for ph in range(depth + 1):
        in_maps = []
        for b in range(Bt):
            for half in range(2):
                core = 2 * b + half
                lyr = ph if half == 0 else ph - 1
                real = 0 <= lyr < depth
                lc = min(max(lyr, 0), depth - 1)
                in_maps.append({
                    "xin": xs[core], "pos": poss[core], "wts": layer_w[lc][None, :],
                    "pa": layer_pa[lc][None], "gains": layer_ga[lc][None], "cst": CST_NP, "nfin": nfin,
                    "st_in": st_zero if half == 0 else st_prev[b],
                    "upd": np.full((128, 1), 1.0 if real else 0.0, np.float32),
                })
        res = run_bass_kernel_spmd(nc, in_maps, core_ids=list(range(ncores)))
        for b in range(Bt):
            for half in range(2):
                core = 2 * b + half
                lyr = ph if half == 0 else ph - 1
                if 0 <= lyr < depth:
                    xs[core] = np.asarray(res.results[core]["xout"])
                    if lyr == depth - 1:
                        ys[core] = np.asarray(res.results[core]["yout"])
            st_prev[b] = np.asarray(res.results[2 * b]["st_out"])
    out = np.zeros((Bt, Sq, D), np.float32)
    for b in range(Bt):
        for half in range(2):
            core = 2 * b + half
            out[b, half * T:(half + 1) * T, :] = ys[core].reshape(D, T).T
    return out


def kernel(x, positions, norm_mix, w_in, ml_conv_w, ml_conv_b, ml_gate_bias, ml_norm, ssd_conv_w, ssd_conv_b,
           ssd_dt_bias, ssd_a_log, ssd_d, ssd_norm, ret_norm, w_out, norm_ffn, w_gate_up, w_down, norm_final):
    x = np.asarray(x, np.float32)
    positions = np.asarray(positions)
    f = lambda a: np.asarray(a, np.float32)
    depth = w_in.shape[0]
    layer_w = [pack_weights(f(w_in[l]), f(w_out[l]), f(w_gate_up[l]), f(w_down[l])) for l in range(depth)]
    layer_pa, layer_ga = [], []
    for l in range(depth):
        pa, ga = pack_params(l, f(norm_mix), f(ml_conv_w), f(ml_conv_b), f(ml_gate_bias), f(ml_norm), f(ssd_conv_w),
                             f(ssd_conv_b), f(ssd_dt_bias), f(ssd_a_log), f(ssd_d), f(ssd_norm), f(ret_norm),
                             f(norm_ffn))
        layer_pa.append(pa)
        layer_ga.append(ga)
    return run_layers_unfused(x, positions, layer_w, layer_pa, layer_ga, f(norm_final))
```
